# Optimizing a Trainium2 kernel written in Bass

```python
import jax, jax.numpy as jnp
from jax import lax
import numpy as np

D_MODEL = 1024
BATCH = 16
SEQ = 2048
DEPTH = 1

D_PLE = 256
D_CONV = 512
CONV_WIDTH = 31
N_HEADS = 8
HEAD_DIM = 64
D_ATTN = N_HEADS * HEAD_DIM
D_KV = HEAD_DIM
N_IDX_HEADS = 8
IDX_DIM = 64
TOPK_MAX = 256
Q_BLOCK = 128
EPS = 1e-6

COL_SIZES = (
    ("conv_in", 2 * D_CONV),
    ("conv_z", D_CONV),
    ("q", D_ATTN),
    ("k", D_KV),
    ("v", D_KV),
    ("attn_z", D_ATTN),
    ("idx_q", N_IDX_HEADS * IDX_DIM),
    ("idx_k", IDX_DIM),
    ("idx_w", N_IDX_HEADS),
    ("gate_a", D_MODEL),
    ("gate_b", D_MODEL),
)
D_IN_TOTAL = 2 * D_CONV + D_CONV + D_ATTN + 2 * D_KV + D_ATTN + N_IDX_HEADS * IDX_DIM + IDX_DIM + N_IDX_HEADS + 2 * D_MODEL

kernel_name = "hybrid_conformer_dsa_gated_block"


def rms_norm(x, g):
    xf = x.astype(jnp.float32)
    y = xf * lax.rsqrt(jnp.mean(xf * xf, axis=-1, keepdims=True) + EPS) * g.astype(jnp.float32)
    return y.astype(x.dtype)


def layer_norm(x, g, b):
    xf = x.astype(jnp.float32)
    mu = jnp.mean(xf, axis=-1, keepdims=True)
    var = jnp.mean(jnp.square(xf - mu), axis=-1, keepdims=True)
    y = (xf - mu) * lax.rsqrt(var + EPS) * g.astype(jnp.float32) + b.astype(jnp.float32)
    return y.astype(x.dtype)


def split_cols(u):
    out, off = {}, 0
    for name, size in COL_SIZES:
        out[name] = u[..., off:off + size]
        off += size
    return out


def causal_depthwise_conv(u, w, b):
    C = u.shape[-1]
    y = lax.conv_general_dilated(
        u.astype(w.dtype), w.reshape(CONV_WIDTH, 1, C),
        window_strides=(1,), padding=[(CONV_WIDTH - 1, 0)],
        dimension_numbers=("NWC", "WIO", "NWC"), feature_group_count=C)
    return y + b


def conformer_branch(cols, conv_w, conv_b, ln_g, ln_b):
    val, gate = jnp.split(cols["conv_in"], 2, axis=-1)
    u = val * jax.nn.sigmoid(gate)
    c = causal_depthwise_conv(u, conv_w, conv_b)
    c = jax.nn.silu(layer_norm(c, ln_g, ln_b))
    return c * jax.nn.silu(cols["conv_z"])


def dsa_sparse_attention(q, k, v, q_idx, k_idx, w_idx):
    B, L = q.shape[0], q.shape[1]
    n_keep = min(TOPK_MAX, L // 4)
    nb = L // Q_BLOCK
    slopes = 2.0 ** (-8.0 * jnp.arange(1, N_HEADS + 1, dtype=jnp.float32) / N_HEADS)
    key_pos = jnp.arange(L, dtype=jnp.int32)

    def to_blocks(a):
        return jnp.moveaxis(a.reshape((B, nb, Q_BLOCK) + a.shape[2:]), 1, 0)

    def block(args):
        qb, qib, wb, t0 = args
        tq = t0 + jnp.arange(Q_BLOCK, dtype=jnp.int32)
        dots = jnp.einsum("bqhd,bsd->bqhs", qib, k_idx).astype(jnp.float32) * (IDX_DIM ** -0.5)
        w_f = wb.astype(jnp.float32) * (N_IDX_HEADS ** -0.5)
        score = jnp.einsum("bqh,bqhs->bqs", w_f, jax.nn.relu(dots))
        causal = key_pos[None, :] <= tq[:, None]
        score = jnp.where(causal[None], score, -jnp.inf)
        _, sel = lax.top_k(score, n_keep)
        gather = jax.vmap(lambda kv_b, idx_b: kv_b[idx_b])
        k_sel = gather(k, sel)
        v_sel = gather(v, sel)
        logits = jnp.einsum("bqhd,bqkd->bqhk", qb, k_sel).astype(jnp.float32) * (HEAD_DIM ** -0.5)
        dist = (tq[None, :, None] - sel).astype(jnp.float32)
        logits = logits - slopes[None, None, :, None] * dist[:, :, None, :]
        valid = sel <= tq[None, :, None]
        logits = jnp.where(valid[:, :, None, :], logits, -jnp.inf)
        probs = jax.nn.softmax(logits, axis=-1)
        o = jnp.einsum("bqhk,bqkd->bqhd", probs, v_sel.astype(jnp.float32))
        return o.astype(qb.dtype)

    t0s = jnp.arange(nb, dtype=jnp.int32) * Q_BLOCK
    out = lax.map(block, (to_blocks(q), to_blocks(q_idx), to_blocks(w_idx), t0s))
    return jnp.moveaxis(out, 0, 1).reshape(B, L, N_HEADS * HEAD_DIM)


def attention_branch(cols):
    B, L = cols["q"].shape[0], cols["q"].shape[1]
    q = cols["q"].reshape(B, L, N_HEADS, HEAD_DIM)
    q_idx = cols["idx_q"].reshape(B, L, N_IDX_HEADS, IDX_DIM)
    o = dsa_sparse_attention(q, cols["k"], cols["v"], q_idx, cols["idx_k"], cols["idx_w"])
    return o * jax.nn.silu(cols["attn_z"])


def setup_inputs(seed: int = 0) -> dict:
    key = jax.random.key(seed)
    ks = jax.random.split(key, 16)
    f32 = jnp.float32
    nrm = lambda k, shape, scale: jax.random.normal(k, shape, f32) * scale
    return {
        "x": nrm(ks[0], (BATCH, SEQ, D_MODEL), 1.0),
        "p": nrm(ks[1], (DEPTH, BATCH, SEQ, D_PLE), 1.0),
        "norm_g": 1.0 + nrm(ks[2], (DEPTH, D_MODEL), 0.02),
        "w_in": nrm(ks[3], (DEPTH, D_MODEL, D_IN_TOTAL), D_MODEL ** -0.5),
        "conv_w": nrm(ks[4], (DEPTH, CONV_WIDTH, D_CONV), CONV_WIDTH ** -0.5),
        "conv_b": nrm(ks[5], (DEPTH, D_CONV), 0.01),
        "conv_ln_g": 1.0 + nrm(ks[6], (DEPTH, D_CONV), 0.02),
        "conv_ln_b": nrm(ks[7], (DEPTH, D_CONV), 0.01),
        "w_a_out": nrm(ks[8], (DEPTH, D_CONV, D_MODEL), D_CONV ** -0.5),
        "w_b_out": nrm(ks[9], (DEPTH, D_ATTN, D_MODEL), D_ATTN ** -0.5),
        "w_o": nrm(ks[10], (DEPTH, D_MODEL, D_MODEL), D_MODEL ** -0.5),
        "ple_norm_g": 1.0 + nrm(ks[11], (DEPTH, D_MODEL), 0.02),
        "w_ple_gate": nrm(ks[12], (DEPTH, D_MODEL, D_MODEL), D_MODEL ** -0.5),
        "w_ple_proj": nrm(ks[13], (DEPTH, D_PLE, D_MODEL), D_PLE ** -0.5),
        "final_g": 1.0 + nrm(ks[14], (D_MODEL,), 0.02),
    }


def reference(x, p, norm_g, w_in, conv_w, conv_b, conv_ln_g, conv_ln_b, w_a_out, w_b_out,
              w_o, ple_norm_g, w_ple_gate, w_ple_proj, final_g):
    for i in range(DEPTH):
        h = rms_norm(x, norm_g[i])
        cols = split_cols(jnp.einsum("bld,de->ble", h, w_in[i]))
        y_a = conformer_branch(cols, conv_w[i], conv_b[i], conv_ln_g[i], conv_ln_b[i])
        y_b = attention_branch(cols)
        merged = (jax.nn.sigmoid(cols["gate_a"]) * jnp.einsum("blc,cd->bld", y_a, w_a_out[i])
                  + jax.nn.sigmoid(cols["gate_b"]) * jnp.einsum("blc,cd->bld", y_b, w_b_out[i]))
        x = x + jnp.einsum("bld,de->ble", merged, w_o[i])
        ple_gate = jax.nn.sigmoid(jnp.einsum("bld,de->ble", rms_norm(x, ple_norm_g[i]), w_ple_gate[i]))
        x = x + ple_gate * jnp.einsum("blp,pd->bld", p[i], w_ple_proj[i])
    return rms_norm(x, final_g)
```

```python
import numpy as np
import concourse.bass as bass
import concourse.mybir as mybir
from concourse.bass_utils import run_bass_kernel_spmd

F32 = mybir.dt.float32
BF16 = mybir.dt.bfloat16
ALU = mybir.AluOpType
AF = mybir.ActivationFunctionType
AX = mybir.AxisListType

NCORES = 8
D = 1024
L = 2048
BPC = 2
CH = 512
NCL = L // CH
NCHUNK = BPC * NCL
TPC = BPC * L
DIN = 5320
NUNIT = 41
RING = 6
NIT = 18
INTERLEAVE = False
EPS = 1e-6
ENGS = ("pe", "act", "dve", "pool", "sp")


class _Ins:
    __slots__ = ("eng", "fn", "deps", "epoch", "is_dma", "sem", "val", "signal", "dma_waits")

    def __init__(self, eng, fn, epoch, is_dma):
        self.eng = eng
        self.fn = fn
        self.epoch = epoch
        self.is_dma = is_dma
        self.deps = []
        self.sem = None
        self.val = None
        self.signal = False
        self.dma_waits = {}


class Sched:
    def __init__(self, nc):
        self.nc = nc
        self.streams = {e: [] for e in ENGS}
        self.last_write = {}
        self.readers = {}
        self.epoch = 0
        self.dma_cnt = {}

    def next_epoch(self):
        self.epoch += 1

    def op(self, eng, fn, reads=(), writes=(), dma=None):
        ins = _Ins(eng, fn, self.epoch, dma is not None)
        deps = []
        seen = set()

        def add(d):
            if d is not None and id(d) not in seen:
                seen.add(id(d))
                deps.append(d)
        for k in reads:
            add(self.last_write.get(k))
        for k in writes:
            add(self.last_write.get(k))
            for r in self.readers.get(k, ()):
                add(r)
        ins.deps = deps
        for k in writes:
            self.last_write[k] = ins
            self.readers[k] = []
        for k in reads:
            if k not in writes:
                self.readers.setdefault(k, []).append(ins)
        for d in deps:
            if d.is_dma:
                ins.dma_waits[d.sem] = self.dma_cnt[d.sem[1]]
        if dma is not None:
            c = self.dma_cnt.get(dma, 0) + 16
            self.dma_cnt[dma] = c
            ins.sem = ("dma", dma)
            ins.val = c
        self.streams[eng].append(ins)
        return ins

    def emit(self, block_engines):
        nc = self.nc
        for e in ENGS:
            for ins in self.streams[e]:
                for d in ins.deps:
                    if d.eng == "pe" and ins.eng == "pe":
                        continue
                    d.signal = True
        sems = {}

        def get_sem(key):
            if key not in sems:
                sems[key] = nc.alloc_semaphore("s_%s_%s" % (key[0], key[1]))
            return sems[key]
        for e in ENGS:
            cnt = {}
            for ins in self.streams[e]:
                if ins.is_dma:
                    get_sem(ins.sem)
                    continue
                if ins.signal:
                    key = (e, ins.epoch)
                    cnt[key] = cnt.get(key, 0) + 1
                    ins.sem = key
                    ins.val = cnt[key]
                    get_sem(key)
        for e in ENGS:
            stream = self.streams[e]
            if not stream:
                continue

            def body(engine, stream=stream, e=e):
                waited = {}
                for ins in stream:
                    need = {}
                    for d in ins.deps:
                        if d.eng == "pe" and e == "pe":
                            continue
                        v = ins.dma_waits[d.sem] if d.is_dma else d.val
                        if v > need.get(d.sem, 0):
                            need[d.sem] = v
                    for key, v in need.items():
                        if waited.get(key, 0) >= v:
                            continue
                        waited[key] = v
                        engine.wait_ge(sems[key], v)
                    if ins.fn is None:
                        continue
                    bi = ins.fn(engine)
                    if ins.is_dma:
                        bi.then_inc(sems[ins.sem], 16)
                    elif ins.signal:
                        bi.then_inc(sems[ins.sem], 1)
            block_engines[e](body)


def unit_cols():
    o_val, o_gate, o_z, o_q, o_k, o_v, o_az, o_iq, o_ik, o_iw, o_ga, o_gb = (
        0, 512, 1024, 1536, 2048, 2112, 2176, 2688, 3200, 3264, 3272, 4296)
    units = []
    units.append(list(range(o_k, o_k + 64)) + list(range(o_ik, o_ik + 64)))
    for h in range(8):
        units.append(list(range(o_q + 64 * h, o_q + 64 * h + 64)) + list(range(o_iq + 64 * h, o_iq + 64 * h + 64)))
    for i in range(4):
        units.append(list(range(o_val + 128 * i, o_val + 128 * i + 128)))
        units.append(list(range(o_gate + 128 * i, o_gate + 128 * i + 128)))
    for i in range(4):
        units.append(list(range(o_z + 128 * i, o_z + 128 * i + 128)))
    for j in range(4):
        units.append(list(range(o_az + 128 * j, o_az + 128 * j + 128)))
    for e in range(8):
        units.append(list(range(o_ga + 128 * e, o_ga + 128 * e + 128)))
        units.append(list(range(o_gb + 128 * e, o_gb + 128 * e + 128)))
    assert len(units) == NUNIT
    vcols = list(range(o_v, o_v + 64)) + list(range(o_iw, o_iw + 8))
    return units, vcols


def build_nc(nchunk=NCHUNK, debug=None):
    nc = bass.Bass("TRN2", target_bir_lowering=False)
    dt_in = lambda name, shape: nc.dram_tensor(name, list(shape), F32, kind="ExternalInput").ap()
    x_d = dt_in("x", [TPC, D])
    p_d = dt_in("p", [TPC, 256])
    wu_d = dt_in("wu", [NUNIT, 128, 8, 128])
    wv_d = dt_in("wv", [128, 8, 72])
    wa_d = dt_in("wa", [128, 4, 1024])
    wb_d = dt_in("wb", [64, 8, 1024])
    wo_d = dt_in("wo", [128, 8, 1024])
    wg_d = dt_in("wg", [128, 8, 1024])
    wp_d = dt_in("wp", [128, 2, 1024])
    cvec_d = dt_in("cvec", [128, 4, 3])
    gvec_d = dt_in("gvec", [128, 3, 1024])
    ident_d = dt_in("ident", [128, 128])
    cb_d = dt_in("cb", [128, 128])
    augk_d = dt_in("augk", [4, L])
    augq_d = dt_in("augq", [4, 8, CH])
    diag_d = dt_in("diagw", [4, 128, 31, 128])
    ctab_d = dt_in("ctab", [128, NIT + 1])
    dscr = nc.dram_tensor("dscr", [4, 128, 31, 128], BF16).ap()
    y_d = nc.dram_tensor("y", [TPC, D], F32, kind="ExternalOutput").ap()
    dbg_out = {}
    if debug:
        for name, shape in debug.items():
            dbg_out[name] = nc.dram_tensor("dbg_" + name, list(shape), F32, kind="ExternalOutput").ap()

    S = Sched(nc)

    base = [(nc.sbuf_base + 63) // 64 * 64]
    top = nc.sbuf_top

    def alloc(name, shape, dtype, at=None):
        nbytes = int(np.prod(shape[1:])) * (2 if dtype == BF16 else 4)
        nbytes = (nbytes + 63) // 64 * 64
        if at is None:
            off = base[0]
            base[0] += nbytes
            assert base[0] <= top, (name, base[0], top)
        else:
            off = at
        return nc.alloc_sbuf_tensor_at(name, list(shape), dtype, offset=off), off

    WR, _ = alloc("WR", [128, RING, 8, 128], BF16)
    Wv, _ = alloc("Wv", [128, 8, 72], BF16)
    WA, _ = alloc("WA", [128, 4, 1024], BF16)
    WB, _ = alloc("WB", [128, 8, 1024], BF16)
    WO, _ = alloc("WO", [128, 8, 1024], BF16)
    WG, _ = alloc("WG", [128, 8, 1024], BF16)
    WP, _ = alloc("WP", [128, 2, 1024], BF16)
    identb, _ = alloc("identb", [128, 128], BF16)
    identf, _ = alloc("identf", [128, 128], F32)
    bigI, _ = alloc("bigI", [128, 128], BF16)
    CB, _ = alloc("CB", [128, 128], F32)
    onesf, _ = alloc("onesf", [128, 128], F32)
    onesrow, _ = alloc("onesrow", [128, 64], F32)
    Gb, _ = alloc("Gb", [128, 1024], F32)
    cvec, _ = alloc("cvec", [128, 4, 3], F32)
    xin, _ = alloc("xin", [128, 4, 1024], F32)
    hb, hb_off = alloc("hb", [128, 4, 1024], BF16)
    Sc2, _ = alloc("Sc2", [128, 2048], F32, at=hb_off)
    hT, _ = alloc("hT", [128, 8, 512], BF16)
    ubuf, _ = alloc("ubuf", [128, 4, 542], BF16)
    yaT, _ = alloc("yaT", [128, 4, 512], BF16)
    c1, _ = alloc("c1", [128, 4, 512], F32)
    scrt, scr_off = alloc("scrt", [128, 4096], F32)
    Sc, _ = alloc("Sc", [128, 2048], F32, at=scr_off)
    Mk, _ = alloc("Mk", [128, 2048], BF16, at=scr_off + 8192)
    Rl, _ = alloc("Rl", [128, 2, 512], F32, at=scr_off + 12288)
    Jk, _ = alloc("Jk", [128, 2048], BF16, at=scr_off + 12288)
    pin, _ = alloc("pin", [128, 4, 256], F32, at=scr_off)
    obuf, _ = alloc("obuf", [128, 4, 1024], F32, at=scr_off)
    pb, _ = alloc("pb", [128, 4, 256], BF16, at=scr_off + 4096)
    pT, _ = alloc("pT", [128, 2, 512], BF16, at=scr_off + 6144)
    QA, qa_off = alloc("QA", [128, 8, 512], BF16)
    QI, _ = alloc("QI", [128, 8, 512], BF16)
    mrg = QI
    KA, _ = alloc("KA", [128, L], BF16)
    KI, _ = alloc("KI", [128, L], BF16)
    VAf, _ = alloc("VAf", [128, 17 * 66], BF16)
    VA = VAf[:, 0:16 * 66].rearrange("p (k c) -> p k c", c=66)
    idxw, _ = alloc("idxw", [128, 4, 8], F32)
    maskT, mt_off = alloc("maskT", [128, 16, 512], BF16)
    diag = [alloc("diag0", [128, 31, 128], BF16, at=mt_off)[0],
            alloc("diag1", [128, 31, 128], BF16, at=mt_off + 8192)[0]]
    Pb, _ = alloc("Pb", [128, 4, 512], BF16)
    ybT, _ = alloc("ybT", [128, 8, 512], BF16)
    tmpf, _ = alloc("tmpf", [128, 3, 512], F32)
    st, _ = alloc("st", [128, 64], F32)
    bs, _ = alloc("bs", [128, 16], F32)
    ctab, _ = alloc("ctab", [128, NIT + 1], F32)
    wtab, _ = alloc("wtab", [128, NIT + 1], F32)
    wtab2, _ = alloc("wtab2", [128, NIT + 1], F32)
    nwtab2, _ = alloc("nwtab2", [128, NIT + 1], F32)
    bs2, _ = alloc("bs2", [128, 16], F32)

    pm = [nc.alloc_psum_tensor("pm0", [128, 512], F32), nc.alloc_psum_tensor("pm1", [128, 512], F32)]
    psc = [nc.alloc_psum_tensor("psc0", [128, 512], F32), nc.alloc_psum_tensor("psc1", [128, 512], F32)]
    pso = nc.alloc_psum_tensor("pso", [128, 512], F32)
    psx = nc.alloc_psum_tensor("psx", [128, 512], F32)
    psy = nc.alloc_psum_tensor("psy", [128, 512], F32)
    pst = nc.alloc_psum_tensor("pst", [128, 1024], BF16)

    def mm(out, lhsT, rhs, start, stop, reads, writes):
        S.op("pe", lambda e: e.matmul(out, lhsT=lhsT, rhs=rhs, start=start, stop=stop), reads, writes)

    def tr(out, in_, reads, writes):
        S.op("pe", lambda e: e.transpose(out=out, in_=in_, identity=identb[:, :]), list(reads) + ["identb"], writes)

    def act(out, in_, func, reads, writes, bias=None, scale=None, accum=None):
        kw = {}
        if bias is not None:
            kw["bias"] = bias
        if scale is not None:
            kw["scale"] = scale
        if accum is not None:
            kw["accum_out"] = accum
        S.op("act", lambda e: e.activation(out=out, in_=in_, func=func, **kw), reads, writes)

    def tt(eng, out, in0, in1, op, reads, writes):
        S.op(eng, lambda e: e.tensor_tensor(out=out, in0=in0, in1=in1, op=op), reads, writes)

    def ts(eng, out, in0, s1, s2, op0, op1, reads, writes, accum=None):
        if accum is not None:
            S.op(eng, lambda e: e.tensor_scalar(out=out, in0=in0, scalar1=s1, scalar2=s2, op0=op0, op1=op1, accum_out=accum), reads, writes)
        elif op1 is None:
            S.op(eng, lambda e: e.tensor_scalar(out=out, in0=in0, scalar1=s1, scalar2=None, op0=op0), reads, writes)
        else:
            S.op(eng, lambda e: e.tensor_scalar(out=out, in0=in0, scalar1=s1, scalar2=s2, op0=op0, op1=op1), reads, writes)

    def stt(out, in0, scalar, in1, op0, op1, reads, writes):
        S.op("dve", lambda e: e.scalar_tensor_tensor(out=out, in0=in0, scalar=scalar, in1=in1, op0=op0, op1=op1), reads, writes)

    def cp(eng, out, in_, reads, writes):
        if eng == "act":
            S.op("act", lambda e: e.copy(out=out, in_=in_), reads, writes)
        else:
            S.op(eng, lambda e: e.tensor_copy(out=out, in_=in_), reads, writes)

    def memset(eng, ap, val, writes):
        S.op(eng, lambda e: e.memset(ap, val), (), writes)

    def dma(eng, out, in_, reads, writes, sem, **kw):
        return S.op(eng, lambda e: e.dma_start(out=out, in_=in_, **kw), reads, writes, dma=sem)

    out_dmas = []

    def dbg(name, src_ap, reads):
        if name in dbg_out:
            out_dmas.append(dma("pool", dbg_out[name], src_ap, reads, [], "out", max_dma_last_dim=2048))

    CAST = dict(max_dma_last_dim=4096)
    unit_state = {"next": 0}
    total_units = nchunk * NUNIT

    def issue_unit():
        g = unit_state["next"]
        if g >= total_units:
            return
        unit_state["next"] = g + 1
        slot = g % RING
        u = g % NUNIT
        dma("pool", WR[:, slot, :, :], wu_d[u, :, :, :], [], ["WR%d" % slot], "wr%d" % slot, **CAST)

    issue_unit()
    dma("pool", Wv[:, :, :], wv_d[:, :, :], [], ["Wv"], "w_tail", **CAST)
    for _ in range(RING - 1):
        issue_unit()
    cons = {"g": 0}

    def take_unit():
        g = cons["g"]
        cons["g"] = g + 1
        return g % RING

    dma("sp", identf[:, :], ident_d[:, :], [], ["identf"], "consts")
    dma("sp", cvec[:, :, :], cvec_d[:, :, :], [], ["cvec"], "consts")
    dma("sp", CB[:, :], cb_d[:, :], [], ["CB"], "consts")
    dma("sp", ctab[:, :], ctab_d[:, :], [], ["ctab"], "consts")
    cp("dve", identb[:, :], identf[:, :], ["identf"], ["identb"])
    ts("dve", bigI[:, :], identf[:, :], 29952.0, None, ALU.mult, None, ["identf"], ["bigI"])
    memset("dve", onesf[:, :], 1.0 / 512.0, ["onesf"])
    memset("dve", onesrow[:, :], 1.0, ["onesrow"])
    memset("dve", VAf[:, :], 0.0, ["VA"])
    memset("dve", VA[:, :, 64:66], 1.0, ["VA"])
    memset("dve", KA[64:128, :], 0.0, ["KA"])
    memset("dve", QA[64:128, :, :], 0.0, ["QA"])
    memset("dve", KI[0:64, :], 0.0, ["KI"])
    for i4 in range(4):
        dma("pool", dscr[i4], diag_d[i4], [], ["dscr%d" % i4], "consts2", **CAST)
    dma("pool", KA[64:68, :], augk_d[:, :], [], ["KA"], "consts2", **CAST)
    dma("pool", QA[64:68, :, :], augq_d[:, :, :], [], ["QA"], "consts2", **CAST)

    def load_tail_ab():
        dma("pool", WA[:, :, :], wa_d[:, :, :], [], ["WA"], "w_tail", **CAST)
        dma("pool", WB[0:64, :, :], wb_d[:, :, :], [], ["WB"], "w_tail", **CAST)

    def load_tail_rest():
        dma("pool", WO[:, :, :], wo_d[:, :, :], [], ["WO"], "w_tail", **CAST)
        dma("pool", WG[:, :, :], wg_d[:, :, :], [], ["WG"], "w_tail", **CAST)
        dma("pool", WP[:, :, :], wp_d[:, :, :], [], ["WP"], "w_tail", **CAST)

    SQK = ["scrM", "Rl0", "Rl1"]
    pmi = [0]

    def next_pm():
        pmi[0] ^= 1
        return pmi[0]

    def proj_unit(out_rows=128, lhs_cols=(0, 128), release=True, slot=None, bank=None):
        if slot is None:
            slot = take_unit()
        c0, c1_ = lhs_cols
        if bank is None:
            i = next_pm()
            bt, bk = pm[i], "pm%d" % i
        else:
            i = None
            bt, bk = bank
        for k in range(8):
            mm(bt[0:out_rows, :], WR[:, slot, k, c0:c1_], hT[:, k, :], k == 0, k == 7,
               ["WR%d" % slot, "hT_lo", "hT_hi"], [bk])
        if release:
            issue_unit()
        return i, slot

    def rstd4():
        act(st[:, 4:8], st[:, 0:4], AF.Sqrt, ["st"] + STK, ["st"], bias=EPS, scale=1.0 / D)
        S.op("dve", lambda e: e.reciprocal(out=st[:, 8:12], in_=st[:, 4:8]), ["st"], ["st"])

    def load_gain(g):
        dma("sp", Gb[:, :], gvec_d[:, g, :], [], ["G"], "gld")

    def sumsq(j, junk, jkeys):
        if j % 2 == 0:
            act(junk, xin[:, j, :], AF.Square, ["xin"], jkeys + ["st%d" % j], accum=st[:, j:j + 1])
        else:
            S.op("dve", lambda e: e.scalar_tensor_tensor(out=junk, in0=xin[:, j, :], scalar=1.0, in1=xin[:, j, :],
                                                         op0=ALU.mult, op1=ALU.mult, accum_out=st[:, j:j + 1]),
                 ["xin"], jkeys + ["st%d" % j])

    STK = ["st0", "st1", "st2", "st3"]

    def rmsnorm_to_hT(gidx, tag):
        for j in range(4):
            sumsq(j, hb[:, j, :], ["hb%d" % j])
        rstd4()
        for j in range(4):
            stt(hb[:, j, :], xin[:, j, :], st[:, 8 + j:9 + j], Gb[:, :], ALU.mult, ALU.mult,
                ["xin", "st", "G", "hb", "hb%d" % j], ["hb%d" % j])
        load_gain(gidx + 1)
        for j in range(4):
            for k in range(8):
                tr(pst[:, k * 128:(k + 1) * 128], hb[:, j, k * 128:(k + 1) * 128], ["hb", "hb%d" % j], ["pst"])
            pv8 = pst[:, 0:1024].rearrange("p (k c) -> p k c", k=8)
            cp("act", hT[:, :, j * 128:(j + 1) * 128], pv8, ["pst"], ["hT_lo", "hT_hi"])

    for c in range(nchunk):
        S.next_epoch()
        b = c // NCL
        cl = c % NCL
        tok0 = c * CH
        t0b = cl * CH
        first = (c == 0)

        load_gain(0)
        dma("sp", xin[:, :, :], x_d[tok0:tok0 + CH, :].rearrange("(j p) d -> p j d", p=128), [], ["xin"], "xin")
        rmsnorm_to_hT(0, "n1")
        if first:
            dbg("hT", hT[:, :, :], ["hT_lo", "hT_hi"])

        i, _ = proj_unit()
        cp("act", KA[0:64, t0b:t0b + CH], pm[i][0:64, :], ["pm%d" % i], ["KA"])
        cp("dve", KI[64:128, t0b:t0b + CH], pm[i][64:128, :], ["pm%d" % i], ["KI"])
        for j in range(4):
            for k in range(8):
                mm(psx[:, j * 72:(j + 1) * 72], hT[:, k, j * 128:(j + 1) * 128], Wv[:, k, :], k == 0, k == 7,
                   ["hT_lo", "hT_hi", "Wv"], ["psx"])
        pv = psx[:, 0:288].rearrange("p (j c) -> p j c", c=72)
        cp("dve", VA[:, cl * 4:cl * 4 + 4, 0:64], pv[:, :, 0:64], ["psx"], ["VA"])
        cp("dve", idxw[:, :, :], pv[:, :, 64:72], ["psx"], ["idxw"])

        memset("dve", QI[0:64, :, :], 0.0, ["QI"])
        for h in range(8):
            i, _ = proj_unit()
            cp("act", QA[0:64, h, :], pm[i][0:64, :], ["pm%d" % i], ["QA"])
            cp("dve", QI[64:128, h, :], pm[i][64:128, :], ["pm%d" % i], ["QI"])
        if first:
            dbg("KA", KA[0:68, 0:512], ["KA"])
            dbg("QA", QA[0:68, :, :], ["QA"])
            dbg("QI", QI[64:128, :, :], ["QI"])
            dbg("VA", VA[:, 0:4, :], ["VA"])
            dbg("idxw", idxw[:, :, :], ["idxw"])

        def load_diag(i4):
            dma("sp", diag[i4 % 2][:, :, :], dscr[i4], ["dscr%d" % i4], ["mT%d" % (i4 % 2)], "dg%d" % (i4 % 2))
        vg_banks = [((pm[0], "pm0"), (pm[1], "pm1")), ((psc[0], "psc0"), (psc[1], "psc1"))]

        def proj_vg(i4):
            bv, bg = vg_banks[i4 % 2]
            proj_unit(bank=bv)
            proj_unit(bank=bg)

        def s4_gen():
            if cl == 0:
                memset("dve", ubuf[:, :, 0:30], 0.0, ["ubuf"])
            else:
                cp("dve", ubuf[:, :, 0:30], ubuf[:, :, 512:542], ["ubuf"], ["ubuf"])
            load_diag(0)
            load_diag(1)
            proj_vg(0)
            yield
            for i4 in range(4):
                dkey = "mT%d" % (i4 % 2)
                dg = diag[i4 % 2]
                (vt, vk), (gt, gk) = vg_banks[i4 % 2]
                if i4 + 1 < 4:
                    proj_vg(i4 + 1)
                    yield
                tk = "tmpf%d" % (i4 % 2)
                act(tmpf[:, i4 % 2, :], gt[:, :], AF.Sigmoid, [gk], [tk])
                tt("dve", ubuf[:, i4, 30:542], vt[:, :], tmpf[:, i4 % 2, :], ALU.mult, [vk, tk], ["ubuf"])
                yield
                for j in range(31):
                    mm(pso[:, :], dg[:, j, :], ubuf[:, i4, j:j + 512], j == 0, j == 30, [dkey, "ubuf"], ["pso"])
                if i4 + 2 < 4:
                    load_diag(i4 + 2)
                yield
                act(c1[:, i4, :], pso[:, :], AF.Identity, ["pso", "cvec"], ["c1"], bias=cvec[:, i4, 0:1])
                act(tmpf[:, 2, :], pso[:, :], AF.Square, ["pso", "cvec"], ["tmpf2"], bias=cvec[:, i4, 0:1])
                mm(psx[:, :], onesf[:, :], c1[:, i4, :], i4 == 0, i4 == 3, ["onesf", "c1"], ["psx"])
                mm(psy[:, :], onesf[:, :], tmpf[:, 2, :], i4 == 0, i4 == 3, ["onesf", "tmpf2"], ["psy"])
                yield
            act(tmpf[:, 1, :], psx[:, :], AF.Square, ["psx"], ["tmpf1"])
            tt("dve", tmpf[:, 1, :], psy[:, :], tmpf[:, 1, :], ALU.subtract, ["psy", "tmpf1"], ["tmpf1"])
            act(tmpf[:, 1, :], tmpf[:, 1, :], AF.Sqrt, ["tmpf1"], ["tmpf1"], bias=EPS, scale=1.0)
            S.op("dve", lambda e: e.reciprocal(out=tmpf[:, 1, :], in_=tmpf[:, 1, :]), ["tmpf1"], ["tmpf1"])
            yield
            for i4 in range(4):
                iz, _ = proj_unit()
                tt("dve", c1[:, i4, :], c1[:, i4, :], psx[:, :], ALU.subtract, ["c1", "psx"], ["c1"])
                tt("dve", c1[:, i4, :], c1[:, i4, :], tmpf[:, 1, :], ALU.mult, ["c1", "tmpf1"], ["c1"])
                yield
                act(tmpf[:, 0, :], c1[:, i4, :], AF.Silu, ["c1", "cvec"], ["tmpf0"],
                    bias=cvec[:, i4, 2:3], scale=cvec[:, i4, 1:2])
                act(tmpf[:, 2, :], pm[iz][:, :], AF.Silu, ["pm%d" % iz], ["tmpf2"])
                tt("dve", yaT[:, i4, :], tmpf[:, 0, :], tmpf[:, 2, :], ALU.mult, ["tmpf0", "tmpf2"], ["yaT"])
                yield
            for jz in range(4):
                slot = take_unit()
                for hh in range(2):
                    h = 2 * jz + hh
                    i, _ = proj_unit(out_rows=64, lhs_cols=(hh * 64, hh * 64 + 64), release=(hh == 1), slot=slot)
                    act(ybT[0:64, h, :], pm[i][0:64, :], AF.Silu, ["pm%d" % i], ["ybT"])
                    yield

        s4 = s4_gen()

        def s4_step(n):
            for _ in range(n):
                try:
                    next(s4)
                except StopIteration:
                    return

        if first:
            load_tail_ab()
        ri = [0]

        def indexer(j, Sb, skey):
            qt = cl * 4 + j
            nk = (qt + 1) * 128
            nseg = (nk + 511) // 512
            for h in range(8):
                for sg in range(nseg):
                    w = min(512, nk - sg * 512)
                    pi = (h * nseg + sg) % 2
                    mm(psc[pi][:, 0:w], QI[:, h, j * 128:(j + 1) * 128], KI[:, sg * 512:sg * 512 + w],
                       True, True, ["QI", "KI"], ["psc%d" % pi])
                    ri[0] ^= 1
                    r = ri[0]
                    act(Rl[:, r, 0:w], psc[pi][:, 0:w], AF.Relu, ["psc%d" % pi], ["Rl%d" % r])
                    dst = Sb[:, sg * 512:sg * 512 + w]
                    if h == 0:
                        ts("dve", dst, Rl[:, r, 0:w], idxw[:, j, 0:1], None, ALU.mult, None,
                           ["Rl%d" % r, "idxw"], [skey])
                    else:
                        stt(dst, Rl[:, r, 0:w], idxw[:, j, h:h + 1], dst, ALU.mult, ALU.add,
                            ["Rl%d" % r, "idxw", skey], [skey])

        def bracket(Sb, skey, nk, b_, bkey):
            S.op("dve", lambda e: e.tensor_reduce(out=b_[:, 0:1], in_=Sb[:, 0:nk], axis=AX.X, op=ALU.max,
                                                  apply_absolute_value=True), [skey], [bkey])
            ts("dve", b_[:, 2:3], b_[:, 0:1], 2.002, 2e-3, ALU.mult, ALU.add, [bkey], [bkey])
            memset("dve", b_[:, 3:4], 0.0, [bkey])

        def mask_T(j, qt):
            for g0 in range(0, qt + 1, 8):
                g1 = min(qt + 1, g0 + 8)
                for kb in range(g0, g1):
                    tr(pst[:, (kb - g0) * 128:(kb - g0 + 1) * 128], Mk[:, kb * 128:(kb + 1) * 128], ["scrM"], ["pst"])
                mkey = "mT0" if g0 == 0 else "mT1"
                cp("act", maskT[:, g0:g1, j * 128:(j + 1) * 128],
                   pst[:, 0:(g1 - g0) * 128].rearrange("p (k c) -> p k c", c=128), ["pst"], [mkey])

        for pair_i, (jA, jB) in enumerate(((0, 1), (2, 3))):
            qtA, qtB = cl * 4 + jA, cl * 4 + jB
            nkA, nkB = (qtA + 1) * 128, (qtB + 1) * 128
            bis = qtA >= 2
            indexer(jA, Sc, "scrA")
            indexer(jB, Sc2, "hb")
            if bis:
                bracket(Sc, "scrA", nkA, bs, "bs")
                ts("dve", wtab[:, :], ctab[:, :], bs[:, 2:3], None, ALU.mult, None, ["bs", "ctab"], ["wtab"])
                bracket(Sc2, "hb", nkB, bs2, "bs2")
                ts("dve", wtab2[:, :], ctab[:, :], bs2[:, 2:3], None, ALU.mult, None, ["bs2", "ctab"], ["wtab2"])
                ts("dve", nwtab2[:, :], ctab[:, :], bs2[:, 2:3], -1.0, ALU.mult, ALU.mult, ["bs2", "ctab"], ["nwtab2"])
            tt("dve", Sc[:, qtA * 128:nkA], Sc[:, qtA * 128:nkA], CB[:, :], ALU.add, ["scrA", "CB"], ["scrA"])
            tt("dve", Sc2[:, qtB * 128:nkB], Sc2[:, qtB * 128:nkB], CB[:, :], ALU.add, ["hb", "CB"], ["hb"])
            if first and jA == 2:
                dbg("Sc", Sc[:, 0:384], ["scrA"])
            if bis:
                jk = ["Rl0", "Rl1"]
                sbias = float(nkB) - 510.5
                if pair_i == 0 and not INTERLEAVE:
                    s4_step(1000)
                for it in range(NIT):
                    ts("dve", Mk[:, 0:nkA], Sc[:, 0:nkA], bs[:, 3:4], 0.0, ALU.is_ge, ALU.add,
                       ["scrA", "bs", "scrM"], ["scrM", "bs"], accum=bs[:, 4:5])
                    ts("dve", bs[:, 5:6], bs[:, 4:5], 255.5, wtab[:, it:it + 1], ALU.is_ge, ALU.mult, ["bs", "wtab"], ["bs"])
                    nxt = it + 1 if it + 1 < NIT else it
                    stt(bs[:, 3:4], bs[:, 5:6], wtab[:, nxt:nxt + 1], bs[:, 3:4], ALU.subtract, ALU.add, ["bs", "wtab"], ["bs"])
                    act(Jk[:, 0:nkB], Sc2[:, 0:nkB], AF.Sign, ["hb", "bs2"], jk + ["bs2"], bias=bs2[:, 3:4], scale=1.0,
                        accum=bs2[:, 4:5])
                    act(bs2[:, 5:6], bs2[:, 4:5], AF.Sign, ["bs2"], ["bs2"], bias=sbias, scale=1.0)
                    act(bs2[:, 3:4], bs2[:, 5:6], AF.Identity, ["bs2", "nwtab2"], ["bs2"], bias=bs2[:, 3:4],
                        scale=nwtab2[:, it + 1:it + 2])
                    if pair_i == 0 and INTERLEAVE:
                        s4_step(2)
                if pair_i == 0:
                    s4_step(1000)
                act(bs2[:, 6:7], bs2[:, 3:4], AF.Identity, ["bs2", "nwtab2"], ["bs2"], bias=nwtab2[:, NIT:NIT + 1], scale=-1.0)
                ts("dve", Mk[:, 0:nkA], Sc[:, 0:nkA], bs[:, 3:4], 1.0, ALU.is_ge, ALU.subtract, ["scrA", "bs", "scrM"], ["scrM"])
                if first and jA == 2:
                    dbg("thr", bs[:, 0:16], ["bs"])
            else:
                if pair_i == 0:
                    s4_step(1000)
                ts("dve", Mk[:, 0:nkA], Sc[:, 0:nkA], -1e29, 1.0, ALU.is_ge, ALU.subtract, ["scrA", "scrM"], ["scrM"])
            mask_T(jA, qtA)
            if bis:
                ts("dve", Mk[:, 0:nkB], Sc2[:, 0:nkB], bs2[:, 6:7], 1.0, ALU.is_ge, ALU.subtract, ["hb", "bs2", "scrM"], ["scrM"])
            else:
                ts("dve", Mk[:, 0:nkB], Sc2[:, 0:nkB], -1e29, 1.0, ALU.is_ge, ALU.subtract, ["hb", "scrM"], ["scrM"])
            mask_T(jB, qtB)

        if first:
            load_tail_rest()
        nkb = (cl + 1) * 4
        items = [(h, kb) for h in range(8) for kb in range(nkb)]
        nit = len(items)
        accs = [(pso, "pso"), (psy, "psy")]
        bcs = [(psx, "psx"), (psx, "psx")]
        lbk = [(psc[0], "psc0"), (psc[1], "psc1"), (pm[0], "pm0"), (pm[1], "pm1")]
        LOOK = 4
        rsl = [0, 2]

        def q0_of(kb):
            return max(0, kb - cl * 4) * 128

        def emit_L(idx):
            h, kb = items[idx]
            q0 = q0_of(kb)
            lt, lk = lbk[idx % LOOK]
            mm(lt[:, q0:CH], KA[:, kb * 128:(kb + 1) * 128], QA[:, h, q0:CH], True, False,
               ["KA", "QA"], [lk])
            mm(lt[:, q0:CH], bigI[:, :], maskT[:, kb, q0:CH], False, True,
               ["bigI", "mT0" if kb < 8 else "mT1"], [lk])

        def emit_norm(h):
            acc, akey = accs[h % 2]
            bc, bkey = bcs[h % 2]
            r = rsl[h % 2]
            mm(bc[0:64, :], onesrow[64:65, 0:64], tmpf[64:65, r, :], True, True, ["onesrow", "tmpf%d" % r], [bkey])
            tt("dve", tmpf[0:64, 1, :], acc[0:64, :], ybT[0:64, h, :], ALU.mult, [akey, "ybT"], ["tmpf1"])
            tt("dve", ybT[0:64, h, :], tmpf[0:64, 1, :], bc[0:64, :], ALU.mult, ["tmpf1", bkey], ["ybT"])

        deferred = []
        for i_ in range(min(LOOK, nit)):
            emit_L(i_)
        for idx in range(nit):
            h, kb = items[idx]
            q0 = q0_of(kb)
            lt, lk = lbk[idx % LOOK]
            pr = idx % 4
            slope = 2.0 ** (-(h + 1))
            ebias = -slope * CH * cl
            mkey = "mT0" if kb < 8 else "mT1"
            acc, akey = accs[h % 2]
            act(Pb[:, pr, q0:CH], lt[:, q0:CH], AF.Exp, [lk], ["Pb%d" % pr], bias=ebias, scale=0.125)
            if idx + LOOK < nit:
                emit_L(idx + LOOK)
            mm(acc[:, q0:CH], VAf[:, kb * 66:kb * 66 + 128], Pb[:, pr, q0:CH], kb == 0, kb == nkb - 1, ["VA", "Pb%d" % pr], [akey])
            while deferred and deferred[0][0] <= idx:
                emit_norm(deferred.pop(0)[1])
            if kb == nkb - 1:
                r = rsl[h % 2]
                S.op("dve", lambda e, r=r, acc=acc: e.reciprocal(out=tmpf[64:65, r, :], in_=acc[64:65, :]), [akey], ["tmpf%d" % r])
                deferred.append((idx + 3, h))
        while deferred:
            emit_norm(deferred.pop(0)[1])
        if first:
            dbg("yaT", yaT[:, :, :], ["yaT"])
            dbg("ybT", ybT[0:64, :, :], ["ybT"])
            dbg("maskT", maskT[:, 0:4, :], ["mT0"])

        for e8 in range(8):
            iga, _ = proj_unit()
            igb, _ = proj_unit()
            for kc in range(4):
                mm(psx[:, :], WA[:, kc, e8 * 128:(e8 + 1) * 128], yaT[:, kc, :], kc == 0, kc == 3, ["WA", "yaT"], ["psx"])
            for h in range(8):
                mm(psy[:, :], WB[0:64, h, e8 * 128:(e8 + 1) * 128], ybT[0:64, h, :], h == 0, h == 7, ["WB", "ybT"], ["psy"])
            act(tmpf[:, 0, :], pm[iga][:, :], AF.Sigmoid, ["pm%d" % iga], ["tmpf0"])
            act(tmpf[:, 1, :], pm[igb][:, :], AF.Sigmoid, ["pm%d" % igb], ["tmpf1"])
            tt("dve", tmpf[:, 0, :], tmpf[:, 0, :], psx[:, :], ALU.mult, ["tmpf0", "psx"], ["tmpf0"])
            tt("dve", tmpf[:, 1, :], tmpf[:, 1, :], psy[:, :], ALU.mult, ["tmpf1", "psy"], ["tmpf1"])
            tt("dve", mrg[:, e8, :], tmpf[:, 0, :], tmpf[:, 1, :], ALU.add, ["tmpf0", "tmpf1"], ["QI"])
        if first:
            dbg("mrg", mrg[:, :, :], ["QI"])

        for j in range(4):
            for hf in range(2):
                i = next_pm()
                for k in range(8):
                    mm(pm[i][:, :], mrg[:, k, j * 128:(j + 1) * 128], WO[:, k, hf * 512:(hf + 1) * 512], k == 0, k == 7,
                       ["QI", "WO"], ["pm%d" % i])
                tt("dve", xin[:, j, hf * 512:(hf + 1) * 512], xin[:, j, hf * 512:(hf + 1) * 512], pm[i][:, :], ALU.add,
                   ["xin", "pm%d" % i], ["xin"])
        rmsnorm_to_hT(1, "n2")
        dma("sp", pin[:, :, :], p_d[tok0:tok0 + CH, :].rearrange("(j p) d -> p j d", p=128), [], ["scrA"], "pin")
        cp("dve", pb[:, :, :], pin[:, :, :], ["scrA"], ["scrA"])
        for j in range(4):
            for kk in range(2):
                tr(pst[:, (j * 2 + kk) * 128:(j * 2 + kk + 1) * 128], pb[:, j, kk * 128:(kk + 1) * 128], ["scrA"], ["pst"])
        for kk in range(2):
            cp("act", pT[:, kk, :].rearrange("p (j c) -> p j c", j=4),
               pst[:, 0:1024].rearrange("p (j k c) -> p j k c", j=4, k=2)[:, :, kk, :], ["pst"], ["scrA"])
        for j in range(4):
            for hf in range(2):
                i = next_pm()
                for k in range(8):
                    mm(pm[i][:, :], hT[:, k, j * 128:(j + 1) * 128], WG[:, k, hf * 512:(hf + 1) * 512], k == 0, k == 7,
                       ["hT_lo", "hT_hi", "WG"], ["pm%d" % i])
                for kk in range(2):
                    mm(psx[:, :], pT[:, kk, j * 128:(j + 1) * 128], WP[:, kk, hf * 512:(hf + 1) * 512], kk == 0, kk == 1,
                       ["scrA", "WP"], ["psx"])
                act(tmpf[:, 0, :], pm[i][:, :], AF.Sigmoid, ["pm%d" % i], ["tmpf0"])
                tt("dve", tmpf[:, 0, :], tmpf[:, 0, :], psx[:, :], ALU.mult, ["tmpf0", "psx"], ["tmpf0"])
                tt("dve", xin[:, j, hf * 512:(hf + 1) * 512], xin[:, j, hf * 512:(hf + 1) * 512], tmpf[:, 0, :], ALU.add,
                   ["xin", "tmpf0"], ["xin"])
        for j in range(4):
            sumsq(j, hb[:, j, :], ["hb%d" % j])
        rstd4()
        for j in range(4):
            stt(obuf[:, j, :], xin[:, j, :], st[:, 8 + j:9 + j], Gb[:, :], ALU.mult, ALU.mult, ["xin", "st", "G"],
                ["scrA"] + SQK)
        out_dmas.append(dma("sp", y_d[tok0:tok0 + CH, :].rearrange("(j p) d -> p j d", p=128), obuf[:, :, :],
                            ["scrA"] + SQK, [], "out"))

    fin = S.op("sp", None)
    fin.deps = list(out_dmas)
    for d in fin.deps:
        fin.dma_waits[d.sem] = S.dma_cnt["out"]

    with nc.Block() as block:
        S.emit({"pe": block.tensor, "act": block.scalar, "dve": block.vector, "pool": block.gpsimd, "sp": block.sync})
    return nc


def _host_consts():
    ident = np.eye(128, dtype=np.float32)
    tt_, ss_ = np.meshgrid(np.arange(128), np.arange(128), indexing="ij")
    cb = np.where(ss_ <= tt_, 0.0, -1e30).astype(np.float32)
    s = np.arange(L)
    augk = np.stack([s % 128, 128 * (s // 128), np.ones(L), np.ones(L)]).astype(np.float32)
    t = np.arange(CH)
    augq = np.zeros((4, 8, CH), np.float32)
    for h in range(8):
        sl = 2.0 ** (-(h + 1))
        augq[0, h] = 8 * sl
        augq[1, h] = 8 * sl
        augq[2, h] = -8 * sl * (t % 128)
        augq[3, h] = -8 * sl * 128 * (t // 128)
    return ident, cb, augk, augq


def _prep_inputs(x, p, norm_g, w_in, conv_w, conv_b, conv_ln_g, conv_ln_b, w_a_out, w_b_out,
                 w_o, ple_norm_g, w_ple_gate, w_ple_proj, final_g):
    f = lambda a: np.ascontiguousarray(np.asarray(a, dtype=np.float32))
    W = f(w_in)[0]
    units, vcols = unit_cols()
    wu = np.stack([W[:, cols].reshape(8, 128, 128).transpose(1, 0, 2) for cols in units])
    wv = W[:, vcols].reshape(8, 128, 72).transpose(1, 0, 2)
    wa = f(w_a_out)[0].reshape(4, 128, 1024).transpose(1, 0, 2)
    wb = f(w_b_out)[0].reshape(8, 64, 1024).transpose(1, 0, 2)
    wo = f(w_o)[0].reshape(8, 128, 1024).transpose(1, 0, 2)
    wg = f(w_ple_gate)[0].reshape(8, 128, 1024).transpose(1, 0, 2)
    wp = f(w_ple_proj)[0].reshape(2, 128, 1024).transpose(1, 0, 2)
    cw = f(conv_w)[0].reshape(31, 4, 128).transpose(2, 1, 0)
    cvec = np.stack([f(conv_b)[0].reshape(4, 128), f(conv_ln_g)[0].reshape(4, 128),
                     f(conv_ln_b)[0].reshape(4, 128)], axis=-1).transpose(1, 0, 2)
    gvec = np.broadcast_to(np.stack([f(norm_g)[0], f(ple_norm_g)[0], f(final_g)])[None], (128, 3, 1024))
    ident, cb, augk, augq = _host_consts()
    diagw = np.zeros((4, 128, 31, 128), np.float32)
    ar = np.arange(128)
    diagw[:, ar, :, ar] = cw.transpose(0, 1, 2)[ar][:, :, :].transpose(0, 1, 2)
    ctab = np.broadcast_to((2.0 ** -(np.arange(NIT + 1) + 1.0))[None, :], (128, NIT + 1))
    shared = dict(diagw=diagw, ctab=ctab, wu=wu, wv=wv, wa=wa, wb=wb, wo=wo, wg=wg, wp=wp, cvec=cvec, gvec=gvec,
                  ident=ident, cb=cb, augk=augk, augq=augq)
    shared = {k: np.ascontiguousarray(v, dtype=np.float32) for k, v in shared.items()}
    xs = f(x).reshape(NCORES, TPC, D)
    ps = f(p)[0].reshape(NCORES, TPC, 256)
    return [dict(shared, x=xs[i], p=ps[i]) for i in range(NCORES)]


def kernel(**inputs):
    in_maps = _prep_inputs(**inputs)
    nc = build_nc()
    res = run_bass_kernel_spmd(nc, in_maps, core_ids=list(range(NCORES)))
    out = np.stack([np.asarray(r["y"], dtype=np.float32) for r in res.results])
    return out.reshape(16, L, D)
```

```python
import numpy as np
import concourse.bass as bass
import concourse.mybir as mybir
from concourse.bass_utils import run_bass_kernel_spmd

F32 = mybir.dt.float32
BF16 = mybir.dt.bfloat16
ALU = mybir.AluOpType
AF = mybir.ActivationFunctionType
AX = mybir.AxisListType

NCORES = 8
D = 1024
L = 2048
BPC = 2
CH = 512
NCL = L // CH
NCHUNK = BPC * NCL
TPC = BPC * L
DIN = 5320
NUNIT = 41
RING = 6
NIT = 18
INTERLEAVE = False
EPS = 1e-6
ENGS = ("pe", "act", "dve", "pool", "sp")


class _Ins:
    __slots__ = ("eng", "fn", "deps", "epoch", "is_dma", "sem", "val", "signal", "dma_waits")

    def __init__(self, eng, fn, epoch, is_dma):
        self.eng = eng
        self.fn = fn
        self.epoch = epoch
        self.is_dma = is_dma
        self.deps = []
        self.sem = None
        self.val = None
        self.signal = False
        self.dma_waits = {}


class Sched:
    def __init__(self, nc):
        self.nc = nc
        self.streams = {e: [] for e in ENGS}
        self.last_write = {}
        self.readers = {}
        self.epoch = 0
        self.dma_cnt = {}

    def next_epoch(self):
        self.epoch += 1

    def op(self, eng, fn, reads=(), writes=(), dma=None):
        ins = _Ins(eng, fn, self.epoch, dma is not None)
        deps = []
        seen = set()

        def add(d):
            if d is not None and id(d) not in seen:
                seen.add(id(d))
                deps.append(d)
        for k in reads:
            add(self.last_write.get(k))
        for k in writes:
            add(self.last_write.get(k))
            for r in self.readers.get(k, ()):
                add(r)
        ins.deps = deps
        for k in writes:
            self.last_write[k] = ins
            self.readers[k] = []
        for k in reads:
            if k not in writes:
                self.readers.setdefault(k, []).append(ins)
        for d in deps:
            if d.is_dma:
                ins.dma_waits[d.sem] = self.dma_cnt[d.sem[1]]
        if dma is not None:
            c = self.dma_cnt.get(dma, 0) + 16
            self.dma_cnt[dma] = c
            ins.sem = ("dma", dma)
            ins.val = c
        self.streams[eng].append(ins)
        return ins

    def emit(self, block_engines):
        nc = self.nc
        for e in ENGS:
            for ins in self.streams[e]:
                for d in ins.deps:
                    if d.eng == "pe" and ins.eng == "pe":
                        continue
                    d.signal = True
        sems = {}

        def get_sem(key):
            if key not in sems:
                sems[key] = nc.alloc_semaphore("s_%s_%s" % (key[0], key[1]))
            return sems[key]
        for e in ENGS:
            cnt = {}
            for ins in self.streams[e]:
                if ins.is_dma:
                    get_sem(ins.sem)
                    continue
                if ins.signal:
                    key = (e, ins.epoch)
                    cnt[key] = cnt.get(key, 0) + 1
                    ins.sem = key
                    ins.val = cnt[key]
                    get_sem(key)
        for e in ENGS:
            stream = self.streams[e]
            if not stream:
                continue

            def body(engine, stream=stream, e=e):
                waited = {}
                for ins in stream:
                    need = {}
                    for d in ins.deps:
                        if d.eng == "pe" and e == "pe":
                            continue
                        v = ins.dma_waits[d.sem] if d.is_dma else d.val
                        if v > need.get(d.sem, 0):
                            need[d.sem] = v
                    for key, v in need.items():
                        if waited.get(key, 0) >= v:
                            continue
                        waited[key] = v
                        engine.wait_ge(sems[key], v)
                    if ins.fn is None:
                        continue
                    bi = ins.fn(engine)
                    if ins.is_dma:
                        bi.then_inc(sems[ins.sem], 16)
                    elif ins.signal:
                        bi.then_inc(sems[ins.sem], 1)
            block_engines[e](body)


def unit_cols():
    o_val, o_gate, o_z, o_q, o_k, o_v, o_az, o_iq, o_ik, o_iw, o_ga, o_gb = (
        0, 512, 1024, 1536, 2048, 2112, 2176, 2688, 3200, 3264, 3272, 4296)
    units = []
    units.append(list(range(o_k, o_k + 64)) + list(range(o_ik, o_ik + 64)))
    for h in range(8):
        units.append(list(range(o_q + 64 * h, o_q + 64 * h + 64)) + list(range(o_iq + 64 * h, o_iq + 64 * h + 64)))
    for i in range(4):
        units.append(list(range(o_val + 128 * i, o_val + 128 * i + 128)))
        units.append(list(range(o_gate + 128 * i, o_gate + 128 * i + 128)))
    for i in range(4):
        units.append(list(range(o_z + 128 * i, o_z + 128 * i + 128)))
    for j in range(4):
        units.append(list(range(o_az + 128 * j, o_az + 128 * j + 128)))
    for e in range(8):
        units.append(list(range(o_ga + 128 * e, o_ga + 128 * e + 128)))
        units.append(list(range(o_gb + 128 * e, o_gb + 128 * e + 128)))
    assert len(units) == NUNIT
    vcols = list(range(o_v, o_v + 64)) + list(range(o_iw, o_iw + 8))
    return units, vcols


def build_nc(nchunk=NCHUNK, debug=None):
    nc = bass.Bass("TRN2", target_bir_lowering=False)
    dt_in = lambda name, shape: nc.dram_tensor(name, list(shape), F32, kind="ExternalInput").ap()
    x_d = dt_in("x", [TPC, D])
    p_d = dt_in("p", [TPC, 256])
    wu_d = dt_in("wu", [NUNIT, 128, 8, 128])
    wv_d = dt_in("wv", [128, 8, 72])
    wa_d = dt_in("wa", [128, 4, 1024])
    wb_d = dt_in("wb", [64, 8, 1024])
    wo_d = dt_in("wo", [128, 8, 1024])
    wg_d = dt_in("wg", [128, 8, 1024])
    wp_d = dt_in("wp", [128, 2, 1024])
    cvec_d = dt_in("cvec", [128, 4, 3])
    gvec_d = dt_in("gvec", [128, 3, 1024])
    ident_d = dt_in("ident", [128, 128])
    cb_d = dt_in("cb", [128, 128])
    augk_d = dt_in("augk", [4, L])
    augq_d = dt_in("augq", [4, 8, CH])
    diag_d = dt_in("diagw", [4, 128, 31, 128])
    ctab_d = dt_in("ctab", [128, NIT + 1])
    dscr = nc.dram_tensor("dscr", [4, 128, 31, 128], BF16).ap()
    y_d = nc.dram_tensor("y", [TPC, D], F32, kind="ExternalOutput").ap()
    dbg_out = {}
    if debug:
        for name, shape in debug.items():
            dbg_out[name] = nc.dram_tensor("dbg_" + name, list(shape), F32, kind="ExternalOutput").ap()

    S = Sched(nc)

    base = [(nc.sbuf_base + 63) // 64 * 64]
    top = nc.sbuf_top

    def alloc(name, shape, dtype, at=None):
        nbytes = int(np.prod(shape[1:])) * (2 if dtype == BF16 else 4)
        nbytes = (nbytes + 63) // 64 * 64
        if at is None:
            off = base[0]
            base[0] += nbytes
            assert base[0] <= top, (name, base[0], top)
        else:
            off = at
        return nc.alloc_sbuf_tensor_at(name, list(shape), dtype, offset=off), off

    WR, _ = alloc("WR", [128, RING, 8, 128], BF16)
    Wv, _ = alloc("Wv", [128, 8, 72], BF16)
    WA, _ = alloc("WA", [128, 4, 1024], BF16)
    WB, _ = alloc("WB", [128, 8, 1024], BF16)
    WO, _ = alloc("WO", [128, 8, 1024], BF16)
    WG, _ = alloc("WG", [128, 8, 1024], BF16)
    WP, _ = alloc("WP", [128, 2, 1024], BF16)
    identb, _ = alloc("identb", [128, 128], BF16)
    identf, _ = alloc("identf", [128, 128], F32)
    bigI, _ = alloc("bigI", [128, 128], BF16)
    CB, _ = alloc("CB", [128, 128], F32)
    onesf, _ = alloc("onesf", [128, 128], F32)
    onesrow, _ = alloc("onesrow", [128, 64], F32)
    Gb, _ = alloc("Gb", [128, 1024], F32)
    cvec, _ = alloc("cvec", [128, 4, 3], F32)
    xin, _ = alloc("xin", [128, 4, 1024], F32)
    hb, hb_off = alloc("hb", [128, 4, 1024], BF16)
    Sc2, _ = alloc("Sc2", [128, 2048], F32, at=hb_off)
    hT, _ = alloc("hT", [128, 8, 512], BF16)
    ubuf, _ = alloc("ubuf", [128, 4, 542], BF16)
    yaT, _ = alloc("yaT", [128, 4, 512], BF16)
    c1, _ = alloc("c1", [128, 4, 512], F32)
    scrt, scr_off = alloc("scrt", [128, 4096], F32)
    Sc, _ = alloc("Sc", [128, 2048], F32, at=scr_off)
    Mk, _ = alloc("Mk", [128, 2048], BF16, at=scr_off + 8192)
    Rl, _ = alloc("Rl", [128, 2, 512], F32, at=scr_off + 12288)
    MkF, _ = alloc("MkF", [128, 2048], F32, at=scr_off + 8192)
    pin, _ = alloc("pin", [128, 4, 256], F32, at=scr_off)
    obuf, _ = alloc("obuf", [128, 4, 1024], F32, at=scr_off)
    pb, _ = alloc("pb", [128, 4, 256], BF16, at=scr_off + 4096)
    pT, _ = alloc("pT", [128, 2, 512], BF16, at=scr_off + 6144)
    QA, qa_off = alloc("QA", [128, 8, 512], BF16)
    QI, _ = alloc("QI", [128, 8, 512], BF16)
    mrg = QI
    KA, _ = alloc("KA", [128, L], BF16)
    KI, _ = alloc("KI", [128, L], BF16)
    VAf, _ = alloc("VAf", [128, 17 * 66], BF16)
    VA = VAf[:, 0:16 * 66].rearrange("p (k c) -> p k c", c=66)
    idxw, _ = alloc("idxw", [128, 4, 8], F32)
    maskT, mt_off = alloc("maskT", [128, 16, 512], BF16)
    diag = [alloc("diag0", [128, 31, 128], BF16, at=mt_off)[0],
            alloc("diag1", [128, 31, 128], BF16, at=mt_off + 8192)[0]]
    Pb, pb_off = alloc("Pb", [128, 4, 512], BF16)
    Jk, _ = alloc("Jk", [128, 2048], BF16, at=pb_off)
    ybT, _ = alloc("ybT", [128, 8, 512], BF16)
    tmpf, _ = alloc("tmpf", [128, 3, 512], F32)
    st, _ = alloc("st", [128, 64], F32)
    bs, _ = alloc("bs", [128, 16], F32)
    ctab, _ = alloc("ctab", [128, NIT + 1], F32)
    wtab, _ = alloc("wtab", [128, NIT + 1], F32)
    wtab2, _ = alloc("wtab2", [128, NIT + 1], F32)
    nwtab2, _ = alloc("nwtab2", [128, NIT + 1], F32)
    bs2, _ = alloc("bs2", [128, 16], F32)

    pm = [nc.alloc_psum_tensor("pm0", [128, 512], F32), nc.alloc_psum_tensor("pm1", [128, 512], F32)]
    psc = [nc.alloc_psum_tensor("psc0", [128, 512], F32), nc.alloc_psum_tensor("psc1", [128, 512], F32)]
    pso = nc.alloc_psum_tensor("pso", [128, 512], F32)
    psx = nc.alloc_psum_tensor("psx", [128, 512], F32)
    psy = nc.alloc_psum_tensor("psy", [128, 512], F32)
    pst = nc.alloc_psum_tensor("pst", [128, 1024], BF16)

    def mm(out, lhsT, rhs, start, stop, reads, writes):
        S.op("pe", lambda e: e.matmul(out, lhsT=lhsT, rhs=rhs, start=start, stop=stop), reads, writes)

    def tr(out, in_, reads, writes):
        S.op("pe", lambda e: e.transpose(out=out, in_=in_, identity=identb[:, :]), list(reads) + ["identb"], writes)

    def act(out, in_, func, reads, writes, bias=None, scale=None, accum=None):
        kw = {}
        if bias is not None:
            kw["bias"] = bias
        if scale is not None:
            kw["scale"] = scale
        if accum is not None:
            kw["accum_out"] = accum
        S.op("act", lambda e: e.activation(out=out, in_=in_, func=func, **kw), reads, writes)

    def tt(eng, out, in0, in1, op, reads, writes):
        S.op(eng, lambda e: e.tensor_tensor(out=out, in0=in0, in1=in1, op=op), reads, writes)

    def ts(eng, out, in0, s1, s2, op0, op1, reads, writes, accum=None):
        if accum is not None:
            S.op(eng, lambda e: e.tensor_scalar(out=out, in0=in0, scalar1=s1, scalar2=s2, op0=op0, op1=op1, accum_out=accum), reads, writes)
        elif op1 is None:
            S.op(eng, lambda e: e.tensor_scalar(out=out, in0=in0, scalar1=s1, scalar2=None, op0=op0), reads, writes)
        else:
            S.op(eng, lambda e: e.tensor_scalar(out=out, in0=in0, scalar1=s1, scalar2=s2, op0=op0, op1=op1), reads, writes)

    def stt(out, in0, scalar, in1, op0, op1, reads, writes):
        S.op("dve", lambda e: e.scalar_tensor_tensor(out=out, in0=in0, scalar=scalar, in1=in1, op0=op0, op1=op1), reads, writes)

    def cp(eng, out, in_, reads, writes):
        if eng == "act":
            S.op("act", lambda e: e.copy(out=out, in_=in_), reads, writes)
        else:
            S.op(eng, lambda e: e.tensor_copy(out=out, in_=in_), reads, writes)

    def memset(eng, ap, val, writes):
        S.op(eng, lambda e: e.memset(ap, val), (), writes)

    def dma(eng, out, in_, reads, writes, sem, **kw):
        return S.op(eng, lambda e: e.dma_start(out=out, in_=in_, **kw), reads, writes, dma=sem)

    out_dmas = []

    def dbg(name, src_ap, reads):
        if name in dbg_out:
            out_dmas.append(dma("pool", dbg_out[name], src_ap, reads, [], "out", max_dma_last_dim=2048))

    CAST = dict(max_dma_last_dim=4096)
    unit_state = {"next": 0}
    total_units = nchunk * NUNIT

    def issue_unit():
        g = unit_state["next"]
        if g >= total_units:
            return
        unit_state["next"] = g + 1
        slot = g % RING
        u = g % NUNIT
        dma("pool", WR[:, slot, :, :], wu_d[u, :, :, :], [], ["WR%d" % slot], "wr%d" % slot, **CAST)

    issue_unit()
    dma("pool", Wv[:, :, :], wv_d[:, :, :], [], ["Wv"], "w_tail", **CAST)
    for _ in range(RING - 1):
        issue_unit()
    cons = {"g": 0}

    def take_unit():
        g = cons["g"]
        cons["g"] = g + 1
        return g % RING

    dma("sp", identf[:, :], ident_d[:, :], [], ["identf"], "consts")
    dma("sp", cvec[:, :, :], cvec_d[:, :, :], [], ["cvec"], "consts")
    dma("sp", CB[:, :], cb_d[:, :], [], ["CB"], "consts")
    dma("sp", ctab[:, :], ctab_d[:, :], [], ["ctab"], "consts")
    cp("dve", identb[:, :], identf[:, :], ["identf"], ["identb"])
    ts("dve", bigI[:, :], identf[:, :], 29952.0, None, ALU.mult, None, ["identf"], ["bigI"])
    memset("dve", onesf[:, :], 1.0 / 512.0, ["onesf"])
    memset("dve", onesrow[:, :], 1.0, ["onesrow"])
    memset("dve", VAf[:, :], 0.0, ["VA"])
    memset("dve", VA[:, :, 64:66], 1.0, ["VA"])
    memset("dve", KA[64:128, :], 0.0, ["KA"])
    memset("dve", QA[64:128, :, :], 0.0, ["QA"])
    memset("dve", KI[0:64, :], 0.0, ["KI"])
    for i4 in range(4):
        dma("pool", dscr[i4], diag_d[i4], [], ["dscr%d" % i4], "consts2", **CAST)
    dma("pool", KA[64:68, :], augk_d[:, :], [], ["KA"], "consts2", **CAST)
    dma("pool", QA[64:68, :, :], augq_d[:, :, :], [], ["QA"], "consts2", **CAST)

    def load_tail_ab():
        dma("pool", WA[:, :, :], wa_d[:, :, :], [], ["WA"], "w_tail", **CAST)
        dma("pool", WB[0:64, :, :], wb_d[:, :, :], [], ["WB"], "w_tail", **CAST)

    def load_tail_rest():
        dma("pool", WO[:, :, :], wo_d[:, :, :], [], ["WO"], "w_tail", **CAST)
        dma("pool", WG[:, :, :], wg_d[:, :, :], [], ["WG"], "w_tail", **CAST)
        dma("pool", WP[:, :, :], wp_d[:, :, :], [], ["WP"], "w_tail", **CAST)

    SQK = ["scrM", "Rl0", "Rl1"]
    pmi = [0]

    def next_pm():
        pmi[0] ^= 1
        return pmi[0]

    def proj_unit(out_rows=128, lhs_cols=(0, 128), release=True, slot=None, bank=None):
        if slot is None:
            slot = take_unit()
        c0, c1_ = lhs_cols
        if bank is None:
            i = next_pm()
            bt, bk = pm[i], "pm%d" % i
        else:
            i = None
            bt, bk = bank
        for k in range(8):
            mm(bt[0:out_rows, :], WR[:, slot, k, c0:c1_], hT[:, k, :], k == 0, k == 7,
               ["WR%d" % slot, "hT_lo", "hT_hi"], [bk])
        if release:
            issue_unit()
        return i, slot

    def rstd4():
        act(st[:, 4:8], st[:, 0:4], AF.Sqrt, ["st"] + STK, ["st"], bias=EPS, scale=1.0 / D)
        S.op("dve", lambda e: e.reciprocal(out=st[:, 8:12], in_=st[:, 4:8]), ["st"], ["st"])

    def load_gain(g):
        dma("sp", Gb[:, :], gvec_d[:, g, :], [], ["G"], "gld")

    def sumsq(j, junk, jkeys):
        if j % 2 == 0:
            act(junk, xin[:, j, :], AF.Square, ["xin"], jkeys + ["st%d" % j], accum=st[:, j:j + 1])
        else:
            S.op("dve", lambda e: e.scalar_tensor_tensor(out=junk, in0=xin[:, j, :], scalar=1.0, in1=xin[:, j, :],
                                                         op0=ALU.mult, op1=ALU.mult, accum_out=st[:, j:j + 1]),
                 ["xin"], jkeys + ["st%d" % j])

    STK = ["st0", "st1", "st2", "st3"]

    def rmsnorm_to_hT(gidx, tag):
        for j in range(4):
            sumsq(j, hb[:, j, :], ["hb%d" % j])
        rstd4()
        for j in range(4):
            stt(hb[:, j, :], xin[:, j, :], st[:, 8 + j:9 + j], Gb[:, :], ALU.mult, ALU.mult,
                ["xin", "st", "G", "hb", "hb%d" % j], ["hb%d" % j])
        load_gain(gidx + 1)
        for j in range(4):
            for k in range(8):
                tr(pst[:, k * 128:(k + 1) * 128], hb[:, j, k * 128:(k + 1) * 128], ["hb", "hb%d" % j], ["pst"])
            pv8 = pst[:, 0:1024].rearrange("p (k c) -> p k c", k=8)
            cp("act", hT[:, :, j * 128:(j + 1) * 128], pv8, ["pst"], ["hT_lo", "hT_hi"])

    for c in range(nchunk):
        S.next_epoch()
        b = c // NCL
        cl = c % NCL
        tok0 = c * CH
        t0b = cl * CH
        first = (c == 0)

        load_gain(0)
        dma("sp", xin[:, :, :], x_d[tok0:tok0 + CH, :].rearrange("(j p) d -> p j d", p=128), [], ["xin"], "xin")
        rmsnorm_to_hT(0, "n1")
        if first:
            dbg("hT", hT[:, :, :], ["hT_lo", "hT_hi"])

        i, _ = proj_unit()
        cp("act", KA[0:64, t0b:t0b + CH], pm[i][0:64, :], ["pm%d" % i], ["KA"])
        cp("dve", KI[64:128, t0b:t0b + CH], pm[i][64:128, :], ["pm%d" % i], ["KI"])
        for j in range(4):
            for k in range(8):
                mm(psx[:, j * 72:(j + 1) * 72], hT[:, k, j * 128:(j + 1) * 128], Wv[:, k, :], k == 0, k == 7,
                   ["hT_lo", "hT_hi", "Wv"], ["psx"])
        pv = psx[:, 0:288].rearrange("p (j c) -> p j c", c=72)
        cp("dve", VA[:, cl * 4:cl * 4 + 4, 0:64], pv[:, :, 0:64], ["psx"], ["VA"])
        cp("dve", idxw[:, :, :], pv[:, :, 64:72], ["psx"], ["idxw"])

        memset("dve", QI[0:64, :, :], 0.0, ["QI"])
        for h in range(8):
            i, _ = proj_unit()
            cp("act", QA[0:64, h, :], pm[i][0:64, :], ["pm%d" % i], ["QA"])
            cp("dve", QI[64:128, h, :], pm[i][64:128, :], ["pm%d" % i], ["QI"])
        if first:
            dbg("KA", KA[0:68, 0:512], ["KA"])
            dbg("QA", QA[0:68, :, :], ["QA"])
            dbg("QI", QI[64:128, :, :], ["QI"])
            dbg("VA", VA[:, 0:4, :], ["VA"])
            dbg("idxw", idxw[:, :, :], ["idxw"])

        def load_diag(i4):
            dma("sp", diag[i4 % 2][:, :, :], dscr[i4], ["dscr%d" % i4], ["mT%d" % (i4 % 2)], "dg%d" % (i4 % 2))
        vg_banks = [((pm[0], "pm0"), (pm[1], "pm1")), ((psc[0], "psc0"), (psc[1], "psc1"))]

        def proj_vg(i4):
            bv, bg = vg_banks[i4 % 2]
            proj_unit(bank=bv)
            proj_unit(bank=bg)

        def s4_gen():
            if cl == 0:
                memset("dve", ubuf[:, :, 0:30], 0.0, ["ubuf"])
            else:
                cp("dve", ubuf[:, :, 0:30], ubuf[:, :, 512:542], ["ubuf"], ["ubuf"])
            load_diag(0)
            load_diag(1)
            proj_vg(0)
            yield
            for i4 in range(4):
                dkey = "mT%d" % (i4 % 2)
                dg = diag[i4 % 2]
                (vt, vk), (gt, gk) = vg_banks[i4 % 2]
                if i4 + 1 < 4:
                    proj_vg(i4 + 1)
                    yield
                tk = "tmpf%d" % (i4 % 2)
                act(tmpf[:, i4 % 2, :], gt[:, :], AF.Sigmoid, [gk], [tk])
                tt("dve", ubuf[:, i4, 30:542], vt[:, :], tmpf[:, i4 % 2, :], ALU.mult, [vk, tk], ["ubuf"])
                yield
                for j in range(31):
                    mm(pso[:, :], dg[:, j, :], ubuf[:, i4, j:j + 512], j == 0, j == 30, [dkey, "ubuf"], ["pso"])
                if i4 + 2 < 4:
                    load_diag(i4 + 2)
                yield
                act(c1[:, i4, :], pso[:, :], AF.Identity, ["pso", "cvec"], ["c1"], bias=cvec[:, i4, 0:1])
                act(tmpf[:, 2, :], pso[:, :], AF.Square, ["pso", "cvec"], ["tmpf2"], bias=cvec[:, i4, 0:1])
                mm(psx[:, :], onesf[:, :], c1[:, i4, :], i4 == 0, i4 == 3, ["onesf", "c1"], ["psx"])
                mm(psy[:, :], onesf[:, :], tmpf[:, 2, :], i4 == 0, i4 == 3, ["onesf", "tmpf2"], ["psy"])
                yield
            act(tmpf[:, 1, :], psx[:, :], AF.Square, ["psx"], ["tmpf1"])
            tt("dve", tmpf[:, 1, :], psy[:, :], tmpf[:, 1, :], ALU.subtract, ["psy", "tmpf1"], ["tmpf1"])
            act(tmpf[:, 1, :], tmpf[:, 1, :], AF.Sqrt, ["tmpf1"], ["tmpf1"], bias=EPS, scale=1.0)
            S.op("dve", lambda e: e.reciprocal(out=tmpf[:, 1, :], in_=tmpf[:, 1, :]), ["tmpf1"], ["tmpf1"])
            yield
            for i4 in range(4):
                iz, _ = proj_unit()
                tt("dve", c1[:, i4, :], c1[:, i4, :], psx[:, :], ALU.subtract, ["c1", "psx"], ["c1"])
                tt("dve", c1[:, i4, :], c1[:, i4, :], tmpf[:, 1, :], ALU.mult, ["c1", "tmpf1"], ["c1"])
                yield
                act(tmpf[:, 0, :], c1[:, i4, :], AF.Silu, ["c1", "cvec"], ["tmpf0"],
                    bias=cvec[:, i4, 2:3], scale=cvec[:, i4, 1:2])
                act(tmpf[:, 2, :], pm[iz][:, :], AF.Silu, ["pm%d" % iz], ["tmpf2"])
                tt("dve", yaT[:, i4, :], tmpf[:, 0, :], tmpf[:, 2, :], ALU.mult, ["tmpf0", "tmpf2"], ["yaT"])
                yield
            for jz in range(4):
                slot = take_unit()
                for hh in range(2):
                    h = 2 * jz + hh
                    i, _ = proj_unit(out_rows=64, lhs_cols=(hh * 64, hh * 64 + 64), release=(hh == 1), slot=slot)
                    act(ybT[0:64, h, :], pm[i][0:64, :], AF.Silu, ["pm%d" % i], ["ybT"])
                    yield

        s4 = s4_gen()

        def s4_step(n):
            for _ in range(n):
                try:
                    next(s4)
                except StopIteration:
                    return

        if first:
            load_tail_ab()
        ri = [0]

        def indexer(j, Sb, skey):
            qt = cl * 4 + j
            nk = (qt + 1) * 128
            nseg = (nk + 511) // 512
            for h in range(8):
                for sg in range(nseg):
                    w = min(512, nk - sg * 512)
                    pi = (h * nseg + sg) % 2
                    mm(psc[pi][:, 0:w], QI[:, h, j * 128:(j + 1) * 128], KI[:, sg * 512:sg * 512 + w],
                       True, True, ["QI", "KI"], ["psc%d" % pi])
                    ri[0] ^= 1
                    r = ri[0]
                    act(Rl[:, r, 0:w], psc[pi][:, 0:w], AF.Relu, ["psc%d" % pi], ["Rl%d" % r])
                    dst = Sb[:, sg * 512:sg * 512 + w]
                    if h == 0:
                        ts("dve", dst, Rl[:, r, 0:w], idxw[:, j, 0:1], None, ALU.mult, None,
                           ["Rl%d" % r, "idxw"], [skey])
                    else:
                        stt(dst, Rl[:, r, 0:w], idxw[:, j, h:h + 1], dst, ALU.mult, ALU.add,
                            ["Rl%d" % r, "idxw", skey], [skey])

        def bracket(Sb, skey, nk, b_, bkey):
            S.op("dve", lambda e: e.tensor_reduce(out=b_[:, 0:1], in_=Sb[:, 0:nk], axis=AX.X, op=ALU.max,
                                                  apply_absolute_value=True), [skey], [bkey])
            ts("dve", b_[:, 2:3], b_[:, 0:1], 2.002, 2e-3, ALU.mult, ALU.add, [bkey], [bkey])
            memset("dve", b_[:, 3:4], 0.0, [bkey])

        def mask_T(j, qt):
            for g0 in range(0, qt + 1, 8):
                g1 = min(qt + 1, g0 + 8)
                for kb in range(g0, g1):
                    tr(pst[:, (kb - g0) * 128:(kb - g0 + 1) * 128], Mk[:, kb * 128:(kb + 1) * 128], ["scrM"], ["pst"])
                mkey = "mT0" if g0 == 0 else "mT1"
                cp("act", maskT[:, g0:g1, j * 128:(j + 1) * 128],
                   pst[:, 0:(g1 - g0) * 128].rearrange("p (k c) -> p k c", c=128), ["pst"], [mkey])

        for pair_i, (jA, jB) in enumerate(((0, 1), (2, 3))):
            qtA, qtB = cl * 4 + jA, cl * 4 + jB
            nkA, nkB = (qtA + 1) * 128, (qtB + 1) * 128
            bis = qtA >= 2
            indexer(jA, Sc, "scrA")
            indexer(jB, Sc2, "hb")
            if bis:
                bracket(Sc, "scrA", nkA, bs, "bs")
                ts("dve", wtab[:, :], ctab[:, :], bs[:, 2:3], None, ALU.mult, None, ["bs", "ctab"], ["wtab"])
                bracket(Sc2, "hb", nkB, bs2, "bs2")
                ts("dve", wtab2[:, :], ctab[:, :], bs2[:, 2:3], None, ALU.mult, None, ["bs2", "ctab"], ["wtab2"])
                ts("dve", nwtab2[:, :], ctab[:, :], bs2[:, 2:3], -1.0, ALU.mult, ALU.mult, ["bs2", "ctab"], ["nwtab2"])
            tt("dve", Sc[:, qtA * 128:nkA], Sc[:, qtA * 128:nkA], CB[:, :], ALU.add, ["scrA", "CB"], ["scrA"])
            tt("dve", Sc2[:, qtB * 128:nkB], Sc2[:, qtB * 128:nkB], CB[:, :], ALU.add, ["hb", "CB"], ["hb"])
            if first and jA == 2:
                dbg("Sc", Sc[:, 0:384], ["scrA"])
            if bis:
                jk = ["Pb0", "Pb1", "Pb2", "Pb3"]
                sbias = float(nkB) - 510.5
                if pair_i == 0 and not INTERLEAVE:
                    s4_step(1000)
                for it in range(NIT):
                    ts("dve", MkF[:, 0:nkA], Sc[:, 0:nkA], bs[:, 3:4], 0.0, ALU.is_ge, ALU.add,
                       ["scrA", "bs", "scrM", "Rl0", "Rl1"], ["scrM", "Rl0", "Rl1", "bs"], accum=bs[:, 4:5])
                    ts("dve", bs[:, 5:6], bs[:, 4:5], 255.5, wtab[:, it:it + 1], ALU.is_ge, ALU.mult, ["bs", "wtab"], ["bs"])
                    nxt = it + 1 if it + 1 < NIT else it
                    stt(bs[:, 3:4], bs[:, 5:6], wtab[:, nxt:nxt + 1], bs[:, 3:4], ALU.subtract, ALU.add, ["bs", "wtab"], ["bs"])
                    act(Jk[:, 0:nkB], Sc2[:, 0:nkB], AF.Sign, ["hb", "bs2"], jk + ["bs2"], bias=bs2[:, 3:4], scale=1.0,
                        accum=bs2[:, 4:5])
                    act(bs2[:, 5:6], bs2[:, 4:5], AF.Sign, ["bs2"], ["bs2"], bias=sbias, scale=1.0)
                    act(bs2[:, 3:4], bs2[:, 5:6], AF.Identity, ["bs2", "nwtab2"], ["bs2"], bias=bs2[:, 3:4],
                        scale=nwtab2[:, it + 1:it + 2])
                    if pair_i == 0 and INTERLEAVE:
                        s4_step(2)
                if pair_i == 0:
                    s4_step(1000)
                act(bs2[:, 6:7], bs2[:, 3:4], AF.Identity, ["bs2", "nwtab2"], ["bs2"], bias=nwtab2[:, NIT:NIT + 1], scale=-1.0)
                ts("dve", Mk[:, 0:nkA], Sc[:, 0:nkA], bs[:, 3:4], 1.0, ALU.is_ge, ALU.subtract, ["scrA", "bs", "scrM"], ["scrM"])
                if first and jA == 2:
                    dbg("thr", bs[:, 0:16], ["bs"])
            else:
                if pair_i == 0:
                    s4_step(1000)
                ts("dve", Mk[:, 0:nkA], Sc[:, 0:nkA], -1e29, 1.0, ALU.is_ge, ALU.subtract, ["scrA", "scrM"], ["scrM"])
            mask_T(jA, qtA)
            if bis:
                ts("dve", Mk[:, 0:nkB], Sc2[:, 0:nkB], bs2[:, 6:7], 1.0, ALU.is_ge, ALU.subtract, ["hb", "bs2", "scrM"], ["scrM"])
            else:
                ts("dve", Mk[:, 0:nkB], Sc2[:, 0:nkB], -1e29, 1.0, ALU.is_ge, ALU.subtract, ["hb", "scrM"], ["scrM"])
            mask_T(jB, qtB)

        if first:
            load_tail_rest()
        nkb = (cl + 1) * 4
        items = [(h, kb) for h in range(8) for kb in range(nkb)]
        nit = len(items)
        accs = [(pso, "pso"), (psy, "psy")]
        bcs = [(psx, "psx"), (psx, "psx")]
        lbk = [(psc[0], "psc0"), (psc[1], "psc1"), (pm[0], "pm0"), (pm[1], "pm1")]
        LOOK = 4
        rsl = [0, 2]

        def q0_of(kb):
            return max(0, kb - cl * 4) * 128

        def emit_L(idx):
            h, kb = items[idx]
            q0 = q0_of(kb)
            lt, lk = lbk[idx % LOOK]
            mm(lt[:, q0:CH], KA[:, kb * 128:(kb + 1) * 128], QA[:, h, q0:CH], True, False,
               ["KA", "QA"], [lk])
            mm(lt[:, q0:CH], bigI[:, :], maskT[:, kb, q0:CH], False, True,
               ["bigI", "mT0" if kb < 8 else "mT1"], [lk])

        def emit_norm(h):
            acc, akey = accs[h % 2]
            bc, bkey = bcs[h % 2]
            r = rsl[h % 2]
            mm(bc[0:64, :], onesrow[64:65, 0:64], tmpf[64:65, r, :], True, True, ["onesrow", "tmpf%d" % r], [bkey])
            tt("dve", tmpf[0:64, 1, :], acc[0:64, :], ybT[0:64, h, :], ALU.mult, [akey, "ybT"], ["tmpf1"])
            tt("dve", ybT[0:64, h, :], tmpf[0:64, 1, :], bc[0:64, :], ALU.mult, ["tmpf1", bkey], ["ybT"])

        deferred = []
        for i_ in range(min(LOOK, nit)):
            emit_L(i_)
        for idx in range(nit):
            h, kb = items[idx]
            q0 = q0_of(kb)
            lt, lk = lbk[idx % LOOK]
            pr = idx % 4
            slope = 2.0 ** (-(h + 1))
            ebias = -slope * CH * cl
            mkey = "mT0" if kb < 8 else "mT1"
            acc, akey = accs[h % 2]
            act(Pb[:, pr, q0:CH], lt[:, q0:CH], AF.Exp, [lk], ["Pb%d" % pr], bias=ebias, scale=0.125)
            if idx + LOOK < nit:
                emit_L(idx + LOOK)
            mm(acc[:, q0:CH], VAf[:, kb * 66:kb * 66 + 128], Pb[:, pr, q0:CH], kb == 0, kb == nkb - 1, ["VA", "Pb%d" % pr], [akey])
            while deferred and deferred[0][0] <= idx:
                emit_norm(deferred.pop(0)[1])
            if kb == nkb - 1:
                r = rsl[h % 2]
                S.op("dve", lambda e, r=r, acc=acc: e.reciprocal(out=tmpf[64:65, r, :], in_=acc[64:65, :]), [akey], ["tmpf%d" % r])
                deferred.append((idx + 3, h))
        while deferred:
            emit_norm(deferred.pop(0)[1])
        if first:
            dbg("yaT", yaT[:, :, :], ["yaT"])
            dbg("ybT", ybT[0:64, :, :], ["ybT"])
            dbg("maskT", maskT[:, 0:4, :], ["mT0"])

        for e8 in range(8):
            iga, _ = proj_unit()
            igb, _ = proj_unit()
            for kc in range(4):
                mm(psx[:, :], WA[:, kc, e8 * 128:(e8 + 1) * 128], yaT[:, kc, :], kc == 0, kc == 3, ["WA", "yaT"], ["psx"])
            for h in range(8):
                mm(psy[:, :], WB[0:64, h, e8 * 128:(e8 + 1) * 128], ybT[0:64, h, :], h == 0, h == 7, ["WB", "ybT"], ["psy"])
            act(tmpf[:, 0, :], pm[iga][:, :], AF.Sigmoid, ["pm%d" % iga], ["tmpf0"])
            act(tmpf[:, 1, :], pm[igb][:, :], AF.Sigmoid, ["pm%d" % igb], ["tmpf1"])
            tt("dve", tmpf[:, 0, :], tmpf[:, 0, :], psx[:, :], ALU.mult, ["tmpf0", "psx"], ["tmpf0"])
            tt("dve", tmpf[:, 1, :], tmpf[:, 1, :], psy[:, :], ALU.mult, ["tmpf1", "psy"], ["tmpf1"])
            tt("dve", mrg[:, e8, :], tmpf[:, 0, :], tmpf[:, 1, :], ALU.add, ["tmpf0", "tmpf1"], ["QI"])
        if first:
            dbg("mrg", mrg[:, :, :], ["QI"])

        for j in range(4):
            for hf in range(2):
                i = next_pm()
                for k in range(8):
                    mm(pm[i][:, :], mrg[:, k, j * 128:(j + 1) * 128], WO[:, k, hf * 512:(hf + 1) * 512], k == 0, k == 7,
                       ["QI", "WO"], ["pm%d" % i])
                tt("dve", xin[:, j, hf * 512:(hf + 1) * 512], xin[:, j, hf * 512:(hf + 1) * 512], pm[i][:, :], ALU.add,
                   ["xin", "pm%d" % i], ["xin"])
        rmsnorm_to_hT(1, "n2")
        dma("sp", pin[:, :, :], p_d[tok0:tok0 + CH, :].rearrange("(j p) d -> p j d", p=128), [], ["scrA"], "pin")
        cp("dve", pb[:, :, :], pin[:, :, :], ["scrA"], ["scrA"])
        for j in range(4):
            for kk in range(2):
                tr(pst[:, (j * 2 + kk) * 128:(j * 2 + kk + 1) * 128], pb[:, j, kk * 128:(kk + 1) * 128], ["scrA"], ["pst"])
        for kk in range(2):
            cp("act", pT[:, kk, :].rearrange("p (j c) -> p j c", j=4),
               pst[:, 0:1024].rearrange("p (j k c) -> p j k c", j=4, k=2)[:, :, kk, :], ["pst"], ["scrA"])
        for j in range(4):
            for hf in range(2):
                i = next_pm()
                for k in range(8):
                    mm(pm[i][:, :], hT[:, k, j * 128:(j + 1) * 128], WG[:, k, hf * 512:(hf + 1) * 512], k == 0, k == 7,
                       ["hT_lo", "hT_hi", "WG"], ["pm%d" % i])
                for kk in range(2):
                    mm(psx[:, :], pT[:, kk, j * 128:(j + 1) * 128], WP[:, kk, hf * 512:(hf + 1) * 512], kk == 0, kk == 1,
                       ["scrA", "WP"], ["psx"])
                act(tmpf[:, 0, :], pm[i][:, :], AF.Sigmoid, ["pm%d" % i], ["tmpf0"])
                tt("dve", tmpf[:, 0, :], tmpf[:, 0, :], psx[:, :], ALU.mult, ["tmpf0", "psx"], ["tmpf0"])
                tt("dve", xin[:, j, hf * 512:(hf + 1) * 512], xin[:, j, hf * 512:(hf + 1) * 512], tmpf[:, 0, :], ALU.add,
                   ["xin", "tmpf0"], ["xin"])
        for j in range(4):
            sumsq(j, hb[:, j, :], ["hb%d" % j])
        rstd4()
        for j in range(4):
            stt(obuf[:, j, :], xin[:, j, :], st[:, 8 + j:9 + j], Gb[:, :], ALU.mult, ALU.mult, ["xin", "st", "G"],
                ["scrA"] + SQK)
        out_dmas.append(dma("sp", y_d[tok0:tok0 + CH, :].rearrange("(j p) d -> p j d", p=128), obuf[:, :, :],
                            ["scrA"] + SQK, [], "out"))

    fin = S.op("sp", None)
    fin.deps = list(out_dmas)
    for d in fin.deps:
        fin.dma_waits[d.sem] = S.dma_cnt["out"]

    with nc.Block() as block:
        S.emit({"pe": block.tensor, "act": block.scalar, "dve": block.vector, "pool": block.gpsimd, "sp": block.sync})
    return nc


def _host_consts():
    ident = np.eye(128, dtype=np.float32)
    tt_, ss_ = np.meshgrid(np.arange(128), np.arange(128), indexing="ij")
    cb = np.where(ss_ <= tt_, 0.0, -1e30).astype(np.float32)
    s = np.arange(L)
    augk = np.stack([s % 128, 128 * (s // 128), np.ones(L), np.ones(L)]).astype(np.float32)
    t = np.arange(CH)
    augq = np.zeros((4, 8, CH), np.float32)
    for h in range(8):
        sl = 2.0 ** (-(h + 1))
        augq[0, h] = 8 * sl
        augq[1, h] = 8 * sl
        augq[2, h] = -8 * sl * (t % 128)
        augq[3, h] = -8 * sl * 128 * (t // 128)
    return ident, cb, augk, augq


def _prep_inputs(x, p, norm_g, w_in, conv_w, conv_b, conv_ln_g, conv_ln_b, w_a_out, w_b_out,
                 w_o, ple_norm_g, w_ple_gate, w_ple_proj, final_g):
    f = lambda a: np.ascontiguousarray(np.asarray(a, dtype=np.float32))
    W = f(w_in)[0]
    units, vcols = unit_cols()
    wu = np.stack([W[:, cols].reshape(8, 128, 128).transpose(1, 0, 2) for cols in units])
    wv = W[:, vcols].reshape(8, 128, 72).transpose(1, 0, 2)
    wa = f(w_a_out)[0].reshape(4, 128, 1024).transpose(1, 0, 2)
    wb = f(w_b_out)[0].reshape(8, 64, 1024).transpose(1, 0, 2)
    wo = f(w_o)[0].reshape(8, 128, 1024).transpose(1, 0, 2)
    wg = f(w_ple_gate)[0].reshape(8, 128, 1024).transpose(1, 0, 2)
    wp = f(w_ple_proj)[0].reshape(2, 128, 1024).transpose(1, 0, 2)
    cw = f(conv_w)[0].reshape(31, 4, 128).transpose(2, 1, 0)
    cvec = np.stack([f(conv_b)[0].reshape(4, 128), f(conv_ln_g)[0].reshape(4, 128),
                     f(conv_ln_b)[0].reshape(4, 128)], axis=-1).transpose(1, 0, 2)
    gvec = np.broadcast_to(np.stack([f(norm_g)[0], f(ple_norm_g)[0], f(final_g)])[None], (128, 3, 1024))
    ident, cb, augk, augq = _host_consts()
    diagw = np.zeros((4, 128, 31, 128), np.float32)
    ar = np.arange(128)
    diagw[:, ar, :, ar] = cw.transpose(0, 1, 2)[ar][:, :, :].transpose(0, 1, 2)
    ctab = np.broadcast_to((2.0 ** -(np.arange(NIT + 1) + 1.0))[None, :], (128, NIT + 1))
    shared = dict(diagw=diagw, ctab=ctab, wu=wu, wv=wv, wa=wa, wb=wb, wo=wo, wg=wg, wp=wp, cvec=cvec, gvec=gvec,
                  ident=ident, cb=cb, augk=augk, augq=augq)
    shared = {k: np.ascontiguousarray(v, dtype=np.float32) for k, v in shared.items()}
    xs = f(x).reshape(NCORES, TPC, D)
    ps = f(p)[0].reshape(NCORES, TPC, 256)
    return [dict(shared, x=xs[i], p=ps[i]) for i in range(NCORES)]


def kernel(**inputs):
    in_maps = _prep_inputs(**inputs)
    nc = build_nc()
    res = run_bass_kernel_spmd(nc, in_maps, core_ids=list(range(NCORES)))
    out = np.stack([np.asarray(r["y"], dtype=np.float32) for r in res.results])
    return out.reshape(16, L, D)
```

```python
import numpy as np
import concourse.bass as bass
import concourse.mybir as mybir
from concourse.bass_utils import run_bass_kernel_spmd

F32 = mybir.dt.float32
BF16 = mybir.dt.bfloat16
ALU = mybir.AluOpType
AF = mybir.ActivationFunctionType
AX = mybir.AxisListType

NCORES = 8
D = 1024
L = 2048
BPC = 2
CH = 512
NCL = L // CH
NCHUNK = BPC * NCL
TPC = BPC * L
DIN = 5320
NUNIT = 41
RING = 6
NIT = 18
INTERLEAVE = False
EPS = 1e-6
ENGS = ("pe", "act", "dve", "pool", "sp")


class _Ins:
    __slots__ = ("eng", "fn", "deps", "epoch", "is_dma", "sem", "val", "signal", "dma_waits")

    def __init__(self, eng, fn, epoch, is_dma):
        self.eng = eng
        self.fn = fn
        self.epoch = epoch
        self.is_dma = is_dma
        self.deps = []
        self.sem = None
        self.val = None
        self.signal = False
        self.dma_waits = {}


class Sched:
    def __init__(self, nc):
        self.nc = nc
        self.streams = {e: [] for e in ENGS}
        self.last_write = {}
        self.readers = {}
        self.epoch = 0
        self.dma_cnt = {}

    def next_epoch(self):
        self.epoch += 1

    def op(self, eng, fn, reads=(), writes=(), dma=None):
        ins = _Ins(eng, fn, self.epoch, dma is not None)
        deps = []
        seen = set()

        def add(d):
            if d is not None and id(d) not in seen:
                seen.add(id(d))
                deps.append(d)
        for k in reads:
            add(self.last_write.get(k))
        for k in writes:
            add(self.last_write.get(k))
            for r in self.readers.get(k, ()):
                add(r)
        ins.deps = deps
        for k in writes:
            self.last_write[k] = ins
            self.readers[k] = []
        for k in reads:
            if k not in writes:
                self.readers.setdefault(k, []).append(ins)
        for d in deps:
            if d.is_dma:
                ins.dma_waits[d.sem] = self.dma_cnt[d.sem[1]]
        if dma is not None:
            c = self.dma_cnt.get(dma, 0) + 16
            self.dma_cnt[dma] = c
            ins.sem = ("dma", dma)
            ins.val = c
        self.streams[eng].append(ins)
        return ins

    def emit(self, block_engines):
        nc = self.nc
        for e in ENGS:
            for ins in self.streams[e]:
                for d in ins.deps:
                    if d.eng == "pe" and ins.eng == "pe":
                        continue
                    d.signal = True
        sems = {}

        def get_sem(key):
            if key not in sems:
                sems[key] = nc.alloc_semaphore("s_%s_%s" % (key[0], key[1]))
            return sems[key]
        for e in ENGS:
            cnt = {}
            for ins in self.streams[e]:
                if ins.is_dma:
                    get_sem(ins.sem)
                    continue
                if ins.signal:
                    key = (e, ins.epoch)
                    cnt[key] = cnt.get(key, 0) + 1
                    ins.sem = key
                    ins.val = cnt[key]
                    get_sem(key)
        for e in ENGS:
            stream = self.streams[e]
            if not stream:
                continue

            def body(engine, stream=stream, e=e):
                waited = {}
                for ins in stream:
                    need = {}
                    for d in ins.deps:
                        if d.eng == "pe" and e == "pe":
                            continue
                        v = ins.dma_waits[d.sem] if d.is_dma else d.val
                        if v > need.get(d.sem, 0):
                            need[d.sem] = v
                    for key, v in need.items():
                        if waited.get(key, 0) >= v:
                            continue
                        waited[key] = v
                        engine.wait_ge(sems[key], v)
                    if ins.fn is None:
                        continue
                    bi = ins.fn(engine)
                    if ins.is_dma:
                        bi.then_inc(sems[ins.sem], 16)
                    elif ins.signal:
                        bi.then_inc(sems[ins.sem], 1)
            block_engines[e](body)


def unit_cols():
    o_val, o_gate, o_z, o_q, o_k, o_v, o_az, o_iq, o_ik, o_iw, o_ga, o_gb = (
        0, 512, 1024, 1536, 2048, 2112, 2176, 2688, 3200, 3264, 3272, 4296)
    units = []
    units.append(list(range(o_k, o_k + 64)) + list(range(o_ik, o_ik + 64)))
    for h in range(8):
        units.append(list(range(o_q + 64 * h, o_q + 64 * h + 64)) + list(range(o_iq + 64 * h, o_iq + 64 * h + 64)))
    for i in range(4):
        units.append(list(range(o_val + 128 * i, o_val + 128 * i + 128)))
        units.append(list(range(o_gate + 128 * i, o_gate + 128 * i + 128)))
    for i in range(4):
        units.append(list(range(o_z + 128 * i, o_z + 128 * i + 128)))
    for j in range(4):
        units.append(list(range(o_az + 128 * j, o_az + 128 * j + 128)))
    for e in range(8):
        units.append(list(range(o_ga + 128 * e, o_ga + 128 * e + 128)))
        units.append(list(range(o_gb + 128 * e, o_gb + 128 * e + 128)))
    assert len(units) == NUNIT
    vcols = list(range(o_v, o_v + 64)) + list(range(o_iw, o_iw + 8))
    return units, vcols


def build_nc(nchunk=NCHUNK, debug=None):
    nc = bass.Bass("TRN2", target_bir_lowering=False)
    dt_in = lambda name, shape: nc.dram_tensor(name, list(shape), F32, kind="ExternalInput").ap()
    x_d = dt_in("x", [TPC, D])
    p_d = dt_in("p", [TPC, 256])
    wu_d = dt_in("wu", [NUNIT, 128, 8, 128])
    wv_d = dt_in("wv", [128, 8, 72])
    wa_d = dt_in("wa", [128, 4, 1024])
    wb_d = dt_in("wb", [64, 8, 1024])
    wo_d = dt_in("wo", [128, 8, 1024])
    wg_d = dt_in("wg", [128, 8, 1024])
    wp_d = dt_in("wp", [128, 2, 1024])
    cvec_d = dt_in("cvec", [128, 4, 3])
    gvec_d = dt_in("gvec", [128, 3, 1024])
    ident_d = dt_in("ident", [128, 128])
    cb_d = dt_in("cb", [128, 128])
    augk_d = dt_in("augk", [4, L])
    augq_d = dt_in("augq", [4, 8, CH])
    diag_d = dt_in("diagw", [4, 128, 31, 128])
    ctab_d = dt_in("ctab", [128, NIT + 1])
    dscr = nc.dram_tensor("dscr", [4, 128, 31, 128], BF16).ap()
    y_d = nc.dram_tensor("y", [TPC, D], F32, kind="ExternalOutput").ap()
    dbg_out = {}
    if debug:
        for name, shape in debug.items():
            dbg_out[name] = nc.dram_tensor("dbg_" + name, list(shape), F32, kind="ExternalOutput").ap()

    S = Sched(nc)

    base = [(nc.sbuf_base + 63) // 64 * 64]
    top = nc.sbuf_top

    def alloc(name, shape, dtype, at=None):
        nbytes = int(np.prod(shape[1:])) * (2 if dtype == BF16 else 4)
        nbytes = (nbytes + 63) // 64 * 64
        if at is None:
            off = base[0]
            base[0] += nbytes
            assert base[0] <= top, (name, base[0], top)
        else:
            off = at
        return nc.alloc_sbuf_tensor_at(name, list(shape), dtype, offset=off), off

    WR, _ = alloc("WR", [128, RING, 8, 128], BF16)
    Wv, _ = alloc("Wv", [128, 8, 72], BF16)
    WA, _ = alloc("WA", [128, 4, 1024], BF16)
    WB, _ = alloc("WB", [128, 8, 1024], BF16)
    WO, _ = alloc("WO", [128, 8, 1024], BF16)
    WG, _ = alloc("WG", [128, 8, 1024], BF16)
    WP, _ = alloc("WP", [128, 2, 1024], BF16)
    identb, _ = alloc("identb", [128, 128], BF16)
    identf, _ = alloc("identf", [128, 128], F32)
    bigI, _ = alloc("bigI", [128, 128], BF16)
    CB, _ = alloc("CB", [128, 128], F32)
    onesf, _ = alloc("onesf", [128, 128], F32)
    onesrow, _ = alloc("onesrow", [128, 64], F32)
    Gb, _ = alloc("Gb", [128, 1024], F32)
    cvec, _ = alloc("cvec", [128, 4, 3], F32)
    xin, _ = alloc("xin", [128, 4, 1024], F32)
    hb, hb_off = alloc("hb", [128, 4, 1024], BF16)
    Sc2, _ = alloc("Sc2", [128, 2048], F32, at=hb_off)
    hT, _ = alloc("hT", [128, 8, 512], BF16)
    ubuf, _ = alloc("ubuf", [128, 4, 542], BF16)
    yaT, _ = alloc("yaT", [128, 4, 512], BF16)
    c1, _ = alloc("c1", [128, 4, 512], F32)
    scrt, scr_off = alloc("scrt", [128, 4096], F32)
    Sc, _ = alloc("Sc", [128, 2048], F32, at=scr_off)
    Mk, _ = alloc("Mk", [128, 2048], BF16, at=scr_off + 8192)
    Rl, _ = alloc("Rl", [128, 2, 512], F32, at=scr_off + 12288)
    Jk, _ = alloc("Jk", [128, 2048], BF16, at=scr_off + 12288)
    pin, _ = alloc("pin", [128, 4, 256], F32, at=scr_off)
    obuf, _ = alloc("obuf", [128, 4, 1024], F32, at=scr_off)
    pb, _ = alloc("pb", [128, 4, 256], BF16, at=scr_off + 4096)
    pT, _ = alloc("pT", [128, 2, 512], BF16, at=scr_off + 6144)
    QA, qa_off = alloc("QA", [128, 8, 512], BF16)
    QI, _ = alloc("QI", [128, 8, 512], BF16)
    mrg = QI
    KA, _ = alloc("KA", [128, L], BF16)
    KI, _ = alloc("KI", [128, L], BF16)
    VAf, _ = alloc("VAf", [128, 17 * 66], BF16)
    VA = VAf[:, 0:16 * 66].rearrange("p (k c) -> p k c", c=66)
    idxw, _ = alloc("idxw", [128, 4, 8], F32)
    maskT, mt_off = alloc("maskT", [128, 16, 512], BF16)
    diag = [alloc("diag0", [128, 31, 128], BF16, at=mt_off)[0],
            alloc("diag1", [128, 31, 128], BF16, at=mt_off + 8192)[0]]
    Pb, _ = alloc("Pb", [128, 4, 512], BF16)
    ybT, _ = alloc("ybT", [128, 8, 512], BF16)
    tmpf, _ = alloc("tmpf", [128, 3, 512], F32)
    st, _ = alloc("st", [128, 64], F32)
    bs, _ = alloc("bs", [128, 16], F32)
    ctab, _ = alloc("ctab", [128, NIT + 1], F32)
    wtab, _ = alloc("wtab", [128, NIT + 1], F32)
    wtab2, _ = alloc("wtab2", [128, NIT + 1], F32)
    nwtab2, _ = alloc("nwtab2", [128, NIT + 1], F32)
    bs2, _ = alloc("bs2", [128, 16], F32)

    pm = [nc.alloc_psum_tensor("pm0", [128, 512], F32), nc.alloc_psum_tensor("pm1", [128, 512], F32)]
    psc = [nc.alloc_psum_tensor("psc0", [128, 512], F32), nc.alloc_psum_tensor("psc1", [128, 512], F32)]
    pso = nc.alloc_psum_tensor("pso", [128, 512], F32)
    psx = nc.alloc_psum_tensor("psx", [128, 512], F32)
    psy = nc.alloc_psum_tensor("psy", [128, 512], F32)
    pst = nc.alloc_psum_tensor("pst", [128, 1024], BF16)

    def mm(out, lhsT, rhs, start, stop, reads, writes):
        S.op("pe", lambda e: e.matmul(out, lhsT=lhsT, rhs=rhs, start=start, stop=stop), reads, writes)

    def tr(out, in_, reads, writes):
        S.op("pe", lambda e: e.transpose(out=out, in_=in_, identity=identb[:, :]), list(reads) + ["identb"], writes)

    def act(out, in_, func, reads, writes, bias=None, scale=None, accum=None):
        kw = {}
        if bias is not None:
            kw["bias"] = bias
        if scale is not None:
            kw["scale"] = scale
        if accum is not None:
            kw["accum_out"] = accum
        S.op("act", lambda e: e.activation(out=out, in_=in_, func=func, **kw), reads, writes)

    def tt(eng, out, in0, in1, op, reads, writes):
        S.op(eng, lambda e: e.tensor_tensor(out=out, in0=in0, in1=in1, op=op), reads, writes)

    def ts(eng, out, in0, s1, s2, op0, op1, reads, writes, accum=None):
        if accum is not None:
            S.op(eng, lambda e: e.tensor_scalar(out=out, in0=in0, scalar1=s1, scalar2=s2, op0=op0, op1=op1, accum_out=accum), reads, writes)
        elif op1 is None:
            S.op(eng, lambda e: e.tensor_scalar(out=out, in0=in0, scalar1=s1, scalar2=None, op0=op0), reads, writes)
        else:
            S.op(eng, lambda e: e.tensor_scalar(out=out, in0=in0, scalar1=s1, scalar2=s2, op0=op0, op1=op1), reads, writes)

    def stt(out, in0, scalar, in1, op0, op1, reads, writes):
        S.op("dve", lambda e: e.scalar_tensor_tensor(out=out, in0=in0, scalar=scalar, in1=in1, op0=op0, op1=op1), reads, writes)

    def cp(eng, out, in_, reads, writes):
        if eng == "act":
            S.op("act", lambda e: e.copy(out=out, in_=in_), reads, writes)
        else:
            S.op(eng, lambda e: e.tensor_copy(out=out, in_=in_), reads, writes)

    def memset(eng, ap, val, writes):
        S.op(eng, lambda e: e.memset(ap, val), (), writes)

    def dma(eng, out, in_, reads, writes, sem, **kw):
        return S.op(eng, lambda e: e.dma_start(out=out, in_=in_, **kw), reads, writes, dma=sem)

    out_dmas = []

    def dbg(name, src_ap, reads):
        if name in dbg_out:
            out_dmas.append(dma("pool", dbg_out[name], src_ap, reads, [], "out", max_dma_last_dim=2048))

    CAST = dict(max_dma_last_dim=4096)
    unit_state = {"next": 0}
    total_units = nchunk * NUNIT

    def issue_unit():
        g = unit_state["next"]
        if g >= total_units:
            return
        unit_state["next"] = g + 1
        slot = g % RING
        u = g % NUNIT
        dma("pool", WR[:, slot, :, :], wu_d[u, :, :, :], [], ["WR%d" % slot], "wr%d" % slot, **CAST)

    issue_unit()
    dma("pool", Wv[:, :, :], wv_d[:, :, :], [], ["Wv"], "w_tail", **CAST)
    for _ in range(RING - 1):
        issue_unit()
    cons = {"g": 0}

    def take_unit():
        g = cons["g"]
        cons["g"] = g + 1
        return g % RING

    dma("sp", identf[:, :], ident_d[:, :], [], ["identf"], "consts")
    dma("sp", cvec[:, :, :], cvec_d[:, :, :], [], ["cvec"], "consts")
    dma("sp", CB[:, :], cb_d[:, :], [], ["CB"], "consts")
    dma("sp", ctab[:, :], ctab_d[:, :], [], ["ctab"], "consts")
    cp("dve", identb[:, :], identf[:, :], ["identf"], ["identb"])
    ts("dve", bigI[:, :], identf[:, :], 29952.0, None, ALU.mult, None, ["identf"], ["bigI"])
    memset("dve", onesf[:, :], 1.0 / 512.0, ["onesf"])
    memset("dve", onesrow[:, :], 1.0, ["onesrow"])
    memset("dve", VAf[:, :], 0.0, ["VA"])
    memset("dve", VA[:, :, 64:66], 1.0, ["VA"])
    memset("dve", KA[64:128, :], 0.0, ["KA"])
    memset("dve", QA[64:128, :, :], 0.0, ["QA"])
    memset("dve", KI[0:64, :], 0.0, ["KI"])
    for i4 in range(4):
        dma("pool", dscr[i4], diag_d[i4], [], ["dscr%d" % i4], "consts2", **CAST)
    dma("pool", KA[64:68, :], augk_d[:, :], [], ["KA"], "consts2", **CAST)
    dma("pool", QA[64:68, :, :], augq_d[:, :, :], [], ["QA"], "consts2", **CAST)

    def load_tail_ab():
        dma("pool", WA[:, :, :], wa_d[:, :, :], [], ["WA"], "w_tail", **CAST)
        dma("pool", WB[0:64, :, :], wb_d[:, :, :], [], ["WB"], "w_tail", **CAST)

    def load_tail_rest():
        dma("pool", WO[:, :, :], wo_d[:, :, :], [], ["WO"], "w_tail", **CAST)
        dma("pool", WG[:, :, :], wg_d[:, :, :], [], ["WG"], "w_tail", **CAST)
        dma("pool", WP[:, :, :], wp_d[:, :, :], [], ["WP"], "w_tail", **CAST)

    SQK = ["scrM", "Rl0", "Rl1"]
    pmi = [0]

    def next_pm():
        pmi[0] ^= 1
        return pmi[0]

    def proj_unit(out_rows=128, lhs_cols=(0, 128), release=True, slot=None, bank=None):
        if slot is None:
            slot = take_unit()
        c0, c1_ = lhs_cols
        if bank is None:
            i = next_pm()
            bt, bk = pm[i], "pm%d" % i
        else:
            i = None
            bt, bk = bank
        for k in range(8):
            mm(bt[0:out_rows, :], WR[:, slot, k, c0:c1_], hT[:, k, :], k == 0, k == 7,
               ["WR%d" % slot, "hT0", "hT1", "hT2", "hT3"], [bk])
        if release:
            issue_unit()
        return i, slot

    def rstd4():
        act(st[:, 4:8], st[:, 0:4], AF.Sqrt, ["st"] + STK, ["st"], bias=EPS, scale=1.0 / D)
        S.op("dve", lambda e: e.reciprocal(out=st[:, 8:12], in_=st[:, 4:8]), ["st"], ["st"])

    def load_gain(g):
        dma("sp", Gb[:, :], gvec_d[:, g, :], [], ["G"], "gld")

    def sumsq(j, junk, jkeys):
        if j % 2 == 0:
            act(junk, xin[:, j, :], AF.Square, ["xin"], jkeys + ["st%d" % j], accum=st[:, j:j + 1])
        else:
            S.op("dve", lambda e: e.scalar_tensor_tensor(out=junk, in0=xin[:, j, :], scalar=1.0, in1=xin[:, j, :],
                                                         op0=ALU.mult, op1=ALU.mult, accum_out=st[:, j:j + 1]),
                 ["xin"], jkeys + ["st%d" % j])

    STK = ["st0", "st1", "st2", "st3"]

    def rmsnorm_to_hT(gidx, tag, after=None):
        for j in range(4):
            sumsq(j, hb[:, j, :], ["hb%d" % j])
        rstd4()
        for j in range(4):
            stt(hb[:, j, :], xin[:, j, :], st[:, 8 + j:9 + j], Gb[:, :], ALU.mult, ALU.mult,
                ["xin", "st", "G", "hb", "hb%d" % j], ["hb%d" % j])
        load_gain(gidx + 1)
        for j in range(4):
            for k in range(8):
                tr(pst[:, k * 128:(k + 1) * 128], hb[:, j, k * 128:(k + 1) * 128], ["hb", "hb%d" % j], ["pst"])
            pv8 = pst[:, 0:1024].rearrange("p (k c) -> p k c", k=8)
            cp("act", hT[:, :, j * 128:(j + 1) * 128], pv8, ["pst"], ["hT%d" % j])
            if after is not None and j >= 1:
                after(j - 1)
        if after is not None:
            after(3)

    for c in range(nchunk):
        S.next_epoch()
        b = c // NCL
        cl = c % NCL
        tok0 = c * CH
        t0b = cl * CH
        first = (c == 0)

        load_gain(0)
        dma("sp", xin[:, :, :], x_d[tok0:tok0 + CH, :].rearrange("(j p) d -> p j d", p=128), [], ["xin"], "xin")
        rmsnorm_to_hT(0, "n1")
        if first:
            dbg("hT", hT[:, :, :], ["hT0", "hT1", "hT2", "hT3"])

        i, _ = proj_unit()
        cp("act", KA[0:64, t0b:t0b + CH], pm[i][0:64, :], ["pm%d" % i], ["KA"])
        cp("dve", KI[64:128, t0b:t0b + CH], pm[i][64:128, :], ["pm%d" % i], ["KI"])
        for j in range(4):
            for k in range(8):
                mm(psx[:, j * 72:(j + 1) * 72], hT[:, k, j * 128:(j + 1) * 128], Wv[:, k, :], k == 0, k == 7,
                   ["hT%d" % j, "Wv"], ["psx"])
        pv = psx[:, 0:288].rearrange("p (j c) -> p j c", c=72)
        cp("dve", VA[:, cl * 4:cl * 4 + 4, 0:64], pv[:, :, 0:64], ["psx"], ["VA"])
        cp("dve", idxw[:, :, :], pv[:, :, 64:72], ["psx"], ["idxw"])

        memset("dve", QI[0:64, :, :], 0.0, ["QI"])
        for h in range(8):
            i, _ = proj_unit()
            cp("act", QA[0:64, h, :], pm[i][0:64, :], ["pm%d" % i], ["QA"])
            cp("dve", QI[64:128, h, :], pm[i][64:128, :], ["pm%d" % i], ["QI"])
        if first:
            dbg("KA", KA[0:68, 0:512], ["KA"])
            dbg("QA", QA[0:68, :, :], ["QA"])
            dbg("QI", QI[64:128, :, :], ["QI"])
            dbg("VA", VA[:, 0:4, :], ["VA"])
            dbg("idxw", idxw[:, :, :], ["idxw"])

        def load_diag(i4):
            dma("sp", diag[i4 % 2][:, :, :], dscr[i4], ["dscr%d" % i4], ["mT%d" % (i4 % 2)], "dg%d" % (i4 % 2))
        vg_banks = [((pm[0], "pm0"), (pm[1], "pm1")), ((psc[0], "psc0"), (psc[1], "psc1"))]

        def proj_vg(i4):
            bv, bg = vg_banks[i4 % 2]
            proj_unit(bank=bv)
            proj_unit(bank=bg)

        def s4_gen():
            if cl == 0:
                memset("dve", ubuf[:, :, 0:30], 0.0, ["ubuf"])
            else:
                cp("dve", ubuf[:, :, 0:30], ubuf[:, :, 512:542], ["ubuf"], ["ubuf"])
            load_diag(0)
            load_diag(1)
            proj_vg(0)
            yield
            for i4 in range(4):
                dkey = "mT%d" % (i4 % 2)
                dg = diag[i4 % 2]
                (vt, vk), (gt, gk) = vg_banks[i4 % 2]
                if i4 + 1 < 4:
                    proj_vg(i4 + 1)
                    yield
                tk = "tmpf%d" % (i4 % 2)
                act(tmpf[:, i4 % 2, :], gt[:, :], AF.Sigmoid, [gk], [tk])
                tt("dve", ubuf[:, i4, 30:542], vt[:, :], tmpf[:, i4 % 2, :], ALU.mult, [vk, tk], ["ubuf"])
                yield
                for j in range(31):
                    mm(pso[:, :], dg[:, j, :], ubuf[:, i4, j:j + 512], j == 0, j == 30, [dkey, "ubuf"], ["pso"])
                if i4 + 2 < 4:
                    load_diag(i4 + 2)
                yield
                act(c1[:, i4, :], pso[:, :], AF.Identity, ["pso", "cvec"], ["c1"], bias=cvec[:, i4, 0:1])
                act(tmpf[:, 2, :], pso[:, :], AF.Square, ["pso", "cvec"], ["tmpf2"], bias=cvec[:, i4, 0:1])
                mm(psx[:, :], onesf[:, :], c1[:, i4, :], i4 == 0, i4 == 3, ["onesf", "c1"], ["psx"])
                mm(psy[:, :], onesf[:, :], tmpf[:, 2, :], i4 == 0, i4 == 3, ["onesf", "tmpf2"], ["psy"])
                yield
            act(tmpf[:, 1, :], psx[:, :], AF.Square, ["psx"], ["tmpf1"])
            tt("dve", tmpf[:, 1, :], psy[:, :], tmpf[:, 1, :], ALU.subtract, ["psy", "tmpf1"], ["tmpf1"])
            act(tmpf[:, 1, :], tmpf[:, 1, :], AF.Sqrt, ["tmpf1"], ["tmpf1"], bias=EPS, scale=1.0)
            S.op("dve", lambda e: e.reciprocal(out=tmpf[:, 1, :], in_=tmpf[:, 1, :]), ["tmpf1"], ["tmpf1"])
            yield
            for i4 in range(4):
                iz, _ = proj_unit()
                tt("dve", c1[:, i4, :], c1[:, i4, :], psx[:, :], ALU.subtract, ["c1", "psx"], ["c1"])
                tt("dve", c1[:, i4, :], c1[:, i4, :], tmpf[:, 1, :], ALU.mult, ["c1", "tmpf1"], ["c1"])
                yield
                act(tmpf[:, 0, :], c1[:, i4, :], AF.Silu, ["c1", "cvec"], ["tmpf0"],
                    bias=cvec[:, i4, 2:3], scale=cvec[:, i4, 1:2])
                act(tmpf[:, 2, :], pm[iz][:, :], AF.Silu, ["pm%d" % iz], ["tmpf2"])
                tt("dve", yaT[:, i4, :], tmpf[:, 0, :], tmpf[:, 2, :], ALU.mult, ["tmpf0", "tmpf2"], ["yaT"])
                yield
            for jz in range(4):
                slot = take_unit()
                for hh in range(2):
                    h = 2 * jz + hh
                    i, _ = proj_unit(out_rows=64, lhs_cols=(hh * 64, hh * 64 + 64), release=(hh == 1), slot=slot)
                    act(ybT[0:64, h, :], pm[i][0:64, :], AF.Silu, ["pm%d" % i], ["ybT"])
                    yield

        s4 = s4_gen()

        def s4_step(n):
            for _ in range(n):
                try:
                    next(s4)
                except StopIteration:
                    return

        if first:
            load_tail_ab()
        ri = [0]

        def indexer(j, Sb, skey):
            qt = cl * 4 + j
            nk = (qt + 1) * 128
            nseg = (nk + 511) // 512
            for h in range(8):
                for sg in range(nseg):
                    w = min(512, nk - sg * 512)
                    pi = (h * nseg + sg) % 2
                    mm(psc[pi][:, 0:w], QI[:, h, j * 128:(j + 1) * 128], KI[:, sg * 512:sg * 512 + w],
                       True, True, ["QI", "KI"], ["psc%d" % pi])
                    ri[0] ^= 1
                    r = ri[0]
                    act(Rl[:, r, 0:w], psc[pi][:, 0:w], AF.Relu, ["psc%d" % pi], ["Rl%d" % r])
                    dst = Sb[:, sg * 512:sg * 512 + w]
                    if h == 0:
                        ts("dve", dst, Rl[:, r, 0:w], idxw[:, j, 0:1], None, ALU.mult, None,
                           ["Rl%d" % r, "idxw"], [skey])
                    else:
                        stt(dst, Rl[:, r, 0:w], idxw[:, j, h:h + 1], dst, ALU.mult, ALU.add,
                            ["Rl%d" % r, "idxw", skey], [skey])

        def bracket(Sb, skey, nk, b_, bkey):
            S.op("dve", lambda e: e.tensor_reduce(out=b_[:, 0:1], in_=Sb[:, 0:nk], axis=AX.X, op=ALU.max,
                                                  apply_absolute_value=True), [skey], [bkey])
            ts("dve", b_[:, 2:3], b_[:, 0:1], 2.002, 2e-3, ALU.mult, ALU.add, [bkey], [bkey])
            memset("dve", b_[:, 3:4], 0.0, [bkey])

        def mask_T(j, qt):
            for g0 in range(0, qt + 1, 8):
                g1 = min(qt + 1, g0 + 8)
                for kb in range(g0, g1):
                    tr(pst[:, (kb - g0) * 128:(kb - g0 + 1) * 128], Mk[:, kb * 128:(kb + 1) * 128], ["scrM"], ["pst"])
                mkey = "mT0" if g0 == 0 else "mT1"
                cp("act", maskT[:, g0:g1, j * 128:(j + 1) * 128],
                   pst[:, 0:(g1 - g0) * 128].rearrange("p (k c) -> p k c", c=128), ["pst"], [mkey])

        for pair_i, (jA, jB) in enumerate(((0, 1), (2, 3))):
            qtA, qtB = cl * 4 + jA, cl * 4 + jB
            nkA, nkB = (qtA + 1) * 128, (qtB + 1) * 128
            bis = qtA >= 2
            indexer(jA, Sc, "scrA")
            indexer(jB, Sc2, "hb")
            if bis:
                bracket(Sc, "scrA", nkA, bs, "bs")
                ts("dve", wtab[:, :], ctab[:, :], bs[:, 2:3], None, ALU.mult, None, ["bs", "ctab"], ["wtab"])
                bracket(Sc2, "hb", nkB, bs2, "bs2")
                ts("dve", wtab2[:, :], ctab[:, :], bs2[:, 2:3], None, ALU.mult, None, ["bs2", "ctab"], ["wtab2"])
                ts("dve", nwtab2[:, :], ctab[:, :], bs2[:, 2:3], -1.0, ALU.mult, ALU.mult, ["bs2", "ctab"], ["nwtab2"])
            tt("dve", Sc[:, qtA * 128:nkA], Sc[:, qtA * 128:nkA], CB[:, :], ALU.add, ["scrA", "CB"], ["scrA"])
            tt("dve", Sc2[:, qtB * 128:nkB], Sc2[:, qtB * 128:nkB], CB[:, :], ALU.add, ["hb", "CB"], ["hb"])
            if first and jA == 2:
                dbg("Sc", Sc[:, 0:384], ["scrA"])
            if bis:
                jk = ["Rl0", "Rl1"]
                sbias = float(nkB) - 510.5
                if pair_i == 0 and not INTERLEAVE:
                    s4_step(1000)
                for it in range(NIT):
                    ts("dve", Mk[:, 0:nkA], Sc[:, 0:nkA], bs[:, 3:4], 0.0, ALU.is_ge, ALU.add,
                       ["scrA", "bs", "scrM"], ["scrM", "bs"], accum=bs[:, 4:5])
                    ts("dve", bs[:, 5:6], bs[:, 4:5], 255.5, wtab[:, it:it + 1], ALU.is_ge, ALU.mult, ["bs", "wtab"], ["bs"])
                    nxt = it + 1 if it + 1 < NIT else it
                    stt(bs[:, 3:4], bs[:, 5:6], wtab[:, nxt:nxt + 1], bs[:, 3:4], ALU.subtract, ALU.add, ["bs", "wtab"], ["bs"])
                    act(Jk[:, 0:nkB], Sc2[:, 0:nkB], AF.Sign, ["hb", "bs2"], jk + ["bs2"], bias=bs2[:, 3:4], scale=1.0,
                        accum=bs2[:, 4:5])
                    act(bs2[:, 5:6], bs2[:, 4:5], AF.Sign, ["bs2"], ["bs2"], bias=sbias, scale=1.0)
                    act(bs2[:, 3:4], bs2[:, 5:6], AF.Identity, ["bs2", "nwtab2"], ["bs2"], bias=bs2[:, 3:4],
                        scale=nwtab2[:, it + 1:it + 2])
                    if pair_i == 0 and INTERLEAVE:
                        s4_step(2)
                if pair_i == 0:
                    s4_step(1000)
                act(bs2[:, 6:7], bs2[:, 3:4], AF.Identity, ["bs2", "nwtab2"], ["bs2"], bias=nwtab2[:, NIT:NIT + 1], scale=-1.0)
                ts("dve", Mk[:, 0:nkA], Sc[:, 0:nkA], bs[:, 3:4], 1.0, ALU.is_ge, ALU.subtract, ["scrA", "bs", "scrM"], ["scrM"])
                if first and jA == 2:
                    dbg("thr", bs[:, 0:16], ["bs"])
            else:
                if pair_i == 0:
                    s4_step(1000)
                ts("dve", Mk[:, 0:nkA], Sc[:, 0:nkA], -1e29, 1.0, ALU.is_ge, ALU.subtract, ["scrA", "scrM"], ["scrM"])
            mask_T(jA, qtA)
            if bis:
                ts("dve", Mk[:, 0:nkB], Sc2[:, 0:nkB], bs2[:, 6:7], 1.0, ALU.is_ge, ALU.subtract, ["hb", "bs2", "scrM"], ["scrM"])
            else:
                ts("dve", Mk[:, 0:nkB], Sc2[:, 0:nkB], -1e29, 1.0, ALU.is_ge, ALU.subtract, ["hb", "scrM"], ["scrM"])
            mask_T(jB, qtB)

        if first:
            load_tail_rest()
        nkb = (cl + 1) * 4
        items = [(h, kb) for h in range(8) for kb in range(nkb)]
        nit = len(items)
        accs = [(pso, "pso"), (psy, "psy")]
        bcs = [(psx, "psx"), (psx, "psx")]
        lbk = [(psc[0], "psc0"), (psc[1], "psc1"), (pm[0], "pm0"), (pm[1], "pm1")]
        LOOK = 4
        rsl = [0, 2]

        def q0_of(kb):
            return max(0, kb - cl * 4) * 128

        def emit_L(idx):
            h, kb = items[idx]
            q0 = q0_of(kb)
            lt, lk = lbk[idx % LOOK]
            mm(lt[:, q0:CH], KA[:, kb * 128:(kb + 1) * 128], QA[:, h, q0:CH], True, False,
               ["KA", "QA"], [lk])
            mm(lt[:, q0:CH], bigI[:, :], maskT[:, kb, q0:CH], False, True,
               ["bigI", "mT0" if kb < 8 else "mT1"], [lk])

        def emit_norm(h):
            acc, akey = accs[h % 2]
            bc, bkey = bcs[h % 2]
            r = rsl[h % 2]
            mm(bc[0:64, :], onesrow[64:65, 0:64], tmpf[64:65, r, :], True, True, ["onesrow", "tmpf%d" % r], [bkey])
            tt("dve", tmpf[0:64, 1, :], acc[0:64, :], ybT[0:64, h, :], ALU.mult, [akey, "ybT"], ["tmpf1"])
            tt("dve", ybT[0:64, h, :], tmpf[0:64, 1, :], bc[0:64, :], ALU.mult, ["tmpf1", bkey], ["ybT"])

        deferred = []
        for i_ in range(min(LOOK, nit)):
            emit_L(i_)
        for idx in range(nit):
            h, kb = items[idx]
            q0 = q0_of(kb)
            lt, lk = lbk[idx % LOOK]
            pr = idx % 4
            slope = 2.0 ** (-(h + 1))
            ebias = -slope * CH * cl
            mkey = "mT0" if kb < 8 else "mT1"
            acc, akey = accs[h % 2]
            act(Pb[:, pr, q0:CH], lt[:, q0:CH], AF.Exp, [lk], ["Pb%d" % pr], bias=ebias, scale=0.125)
            if idx + LOOK < nit:
                emit_L(idx + LOOK)
            mm(acc[:, q0:CH], VAf[:, kb * 66:kb * 66 + 128], Pb[:, pr, q0:CH], kb == 0, kb == nkb - 1, ["VA", "Pb%d" % pr], [akey])
            while deferred and deferred[0][0] <= idx:
                emit_norm(deferred.pop(0)[1])
            if kb == nkb - 1:
                r = rsl[h % 2]
                S.op("dve", lambda e, r=r, acc=acc: e.reciprocal(out=tmpf[64:65, r, :], in_=acc[64:65, :]), [akey], ["tmpf%d" % r])
                deferred.append((idx + 3, h))
        while deferred:
            emit_norm(deferred.pop(0)[1])
        if first:
            dbg("yaT", yaT[:, :, :], ["yaT"])
            dbg("ybT", ybT[0:64, :, :], ["ybT"])
            dbg("maskT", maskT[:, 0:4, :], ["mT0"])

        dma("sp", pin[:, :, :], p_d[tok0:tok0 + CH, :].rearrange("(j p) d -> p j d", p=128), [], ["scrA"], "pin")
        cp("dve", pb[:, :, :], pin[:, :, :], ["scrA"], ["scrA"])
        for j in range(4):
            for kk in range(2):
                tr(pst[:, (j * 2 + kk) * 128:(j * 2 + kk + 1) * 128], pb[:, j, kk * 128:(kk + 1) * 128], ["scrA"], ["pst"])
        for kk in range(2):
            cp("act", pT[:, kk, :].rearrange("p (j c) -> p j c", j=4),
               pst[:, 0:1024].rearrange("p (j k c) -> p j k c", j=4, k=2)[:, :, kk, :], ["pst"], ["scrA"])
        for e8 in range(8):
            iga, _ = proj_unit()
            igb, _ = proj_unit()
            for kc in range(4):
                mm(psx[:, :], WA[:, kc, e8 * 128:(e8 + 1) * 128], yaT[:, kc, :], kc == 0, kc == 3, ["WA", "yaT"], ["psx"])
            for h in range(8):
                mm(psy[:, :], WB[0:64, h, e8 * 128:(e8 + 1) * 128], ybT[0:64, h, :], h == 0, h == 7, ["WB", "ybT"], ["psy"])
            act(tmpf[:, 0, :], pm[iga][:, :], AF.Sigmoid, ["pm%d" % iga], ["tmpf0"])
            act(tmpf[:, 1, :], pm[igb][:, :], AF.Sigmoid, ["pm%d" % igb], ["tmpf1"])
            tt("dve", tmpf[:, 0, :], tmpf[:, 0, :], psx[:, :], ALU.mult, ["tmpf0", "psx"], ["tmpf0"])
            tt("dve", tmpf[:, 1, :], tmpf[:, 1, :], psy[:, :], ALU.mult, ["tmpf1", "psy"], ["tmpf1"])
            tt("dve", mrg[:, e8, :], tmpf[:, 0, :], tmpf[:, 1, :], ALU.add, ["tmpf0", "tmpf1"], ["QI"])
        if first:
            dbg("mrg", mrg[:, :, :], ["QI"])

        for j in range(4):
            for hf in range(2):
                i = next_pm()
                for k in range(8):
                    mm(pm[i][:, :], mrg[:, k, j * 128:(j + 1) * 128], WO[:, k, hf * 512:(hf + 1) * 512], k == 0, k == 7,
                       ["QI", "WO"], ["pm%d" % i])
                tt("dve", xin[:, j, hf * 512:(hf + 1) * 512], xin[:, j, hf * 512:(hf + 1) * 512], pm[i][:, :], ALU.add,
                   ["xin", "pm%d" % i], ["xin"])
        def ple_tile(j):
            for hf in range(2):
                i = next_pm()
                for k in range(8):
                    mm(pm[i][:, :], hT[:, k, j * 128:(j + 1) * 128], WG[:, k, hf * 512:(hf + 1) * 512], k == 0, k == 7,
                       ["hT%d" % j, "WG"], ["pm%d" % i])
                for kk in range(2):
                    mm(psx[:, :], pT[:, kk, j * 128:(j + 1) * 128], WP[:, kk, hf * 512:(hf + 1) * 512], kk == 0, kk == 1,
                       ["scrA", "WP"], ["psx"])
                act(tmpf[:, 0, :], pm[i][:, :], AF.Sigmoid, ["pm%d" % i], ["tmpf0"])
                tt("dve", tmpf[:, 0, :], tmpf[:, 0, :], psx[:, :], ALU.mult, ["tmpf0", "psx"], ["tmpf0"])
                tt("dve", xin[:, j, hf * 512:(hf + 1) * 512], xin[:, j, hf * 512:(hf + 1) * 512], tmpf[:, 0, :], ALU.add,
                   ["xin", "tmpf0"], ["xin"])
        rmsnorm_to_hT(1, "n2", after=ple_tile)
        for j in range(4):
            sumsq(j, hb[:, j, :], ["hb%d" % j])
        rstd4()
        for j in range(4):
            stt(obuf[:, j, :], xin[:, j, :], st[:, 8 + j:9 + j], Gb[:, :], ALU.mult, ALU.mult, ["xin", "st", "G"],
                ["scrA"] + SQK)
        out_dmas.append(dma("sp", y_d[tok0:tok0 + CH, :].rearrange("(j p) d -> p j d", p=128), obuf[:, :, :],
                            ["scrA"] + SQK, [], "out"))

    fin = S.op("sp", None)
    fin.deps = list(out_dmas)
    for d in fin.deps:
        fin.dma_waits[d.sem] = S.dma_cnt["out"]

    with nc.Block() as block:
        S.emit({"pe": block.tensor, "act": block.scalar, "dve": block.vector, "pool": block.gpsimd, "sp": block.sync})
    return nc


def _host_consts():
    ident = np.eye(128, dtype=np.float32)
    tt_, ss_ = np.meshgrid(np.arange(128), np.arange(128), indexing="ij")
    cb = np.where(ss_ <= tt_, 0.0, -1e30).astype(np.float32)
    s = np.arange(L)
    augk = np.stack([s % 128, 128 * (s // 128), np.ones(L), np.ones(L)]).astype(np.float32)
    t = np.arange(CH)
    augq = np.zeros((4, 8, CH), np.float32)
    for h in range(8):
        sl = 2.0 ** (-(h + 1))
        augq[0, h] = 8 * sl
        augq[1, h] = 8 * sl
        augq[2, h] = -8 * sl * (t % 128)
        augq[3, h] = -8 * sl * 128 * (t // 128)
    return ident, cb, augk, augq


def _prep_inputs(x, p, norm_g, w_in, conv_w, conv_b, conv_ln_g, conv_ln_b, w_a_out, w_b_out,
                 w_o, ple_norm_g, w_ple_gate, w_ple_proj, final_g):
    f = lambda a: np.ascontiguousarray(np.asarray(a, dtype=np.float32))
    W = f(w_in)[0]
    units, vcols = unit_cols()
    wu = np.stack([W[:, cols].reshape(8, 128, 128).transpose(1, 0, 2) for cols in units])
    wv = W[:, vcols].reshape(8, 128, 72).transpose(1, 0, 2)
    wa = f(w_a_out)[0].reshape(4, 128, 1024).transpose(1, 0, 2)
    wb = f(w_b_out)[0].reshape(8, 64, 1024).transpose(1, 0, 2)
    wo = f(w_o)[0].reshape(8, 128, 1024).transpose(1, 0, 2)
    wg = f(w_ple_gate)[0].reshape(8, 128, 1024).transpose(1, 0, 2)
    wp = f(w_ple_proj)[0].reshape(2, 128, 1024).transpose(1, 0, 2)
    cw = f(conv_w)[0].reshape(31, 4, 128).transpose(2, 1, 0)
    cvec = np.stack([f(conv_b)[0].reshape(4, 128), f(conv_ln_g)[0].reshape(4, 128),
                     f(conv_ln_b)[0].reshape(4, 128)], axis=-1).transpose(1, 0, 2)
    gvec = np.broadcast_to(np.stack([f(norm_g)[0], f(ple_norm_g)[0], f(final_g)])[None], (128, 3, 1024))
    ident, cb, augk, augq = _host_consts()
    diagw = np.zeros((4, 128, 31, 128), np.float32)
    ar = np.arange(128)
    diagw[:, ar, :, ar] = cw.transpose(0, 1, 2)[ar][:, :, :].transpose(0, 1, 2)
    ctab = np.broadcast_to((2.0 ** -(np.arange(NIT + 1) + 1.0))[None, :], (128, NIT + 1))
    shared = dict(diagw=diagw, ctab=ctab, wu=wu, wv=wv, wa=wa, wb=wb, wo=wo, wg=wg, wp=wp, cvec=cvec, gvec=gvec,
                  ident=ident, cb=cb, augk=augk, augq=augq)
    shared = {k: np.ascontiguousarray(v, dtype=np.float32) for k, v in shared.items()}
    xs = f(x).reshape(NCORES, TPC, D)
    ps = f(p)[0].reshape(NCORES, TPC, 256)
    return [dict(shared, x=xs[i], p=ps[i]) for i in range(NCORES)]


def kernel(**inputs):
    in_maps = _prep_inputs(**inputs)
    nc = build_nc()
    res = run_bass_kernel_spmd(nc, in_maps, core_ids=list(range(NCORES)))
    out = np.stack([np.asarray(r["y"], dtype=np.float32) for r in res.results])
    return out.reshape(16, L, D)
```

```python
import numpy as np
import concourse.bass as bass
import concourse.mybir as mybir
from concourse.bass_utils import run_bass_kernel_spmd

F32 = mybir.dt.float32
BF16 = mybir.dt.bfloat16
ALU = mybir.AluOpType
AF = mybir.ActivationFunctionType
AX = mybir.AxisListType

NCORES = 8
D = 1024
L = 2048
BPC = 2
CH = 512
NCL = L // CH
NCHUNK = BPC * NCL
TPC = BPC * L
DIN = 5320
NUNIT = 41
RING = 6
NIT = 16
INTERLEAVE = False
EPS = 1e-6
ENGS = ("pe", "act", "dve", "pool", "sp")


class _Ins:
    __slots__ = ("eng", "fn", "deps", "epoch", "is_dma", "sem", "val", "signal", "dma_waits")

    def __init__(self, eng, fn, epoch, is_dma):
        self.eng = eng
        self.fn = fn
        self.epoch = epoch
        self.is_dma = is_dma
        self.deps = []
        self.sem = None
        self.val = None
        self.signal = False
        self.dma_waits = {}


class Sched:
    def __init__(self, nc):
        self.nc = nc
        self.streams = {e: [] for e in ENGS}
        self.last_write = {}
        self.readers = {}
        self.epoch = 0
        self.dma_cnt = {}

    def next_epoch(self):
        self.epoch += 1

    def op(self, eng, fn, reads=(), writes=(), dma=None):
        ins = _Ins(eng, fn, self.epoch, dma is not None)
        deps = []
        seen = set()

        def add(d):
            if d is not None and id(d) not in seen:
                seen.add(id(d))
                deps.append(d)
        for k in reads:
            add(self.last_write.get(k))
        for k in writes:
            add(self.last_write.get(k))
            for r in self.readers.get(k, ()):
                add(r)
        ins.deps = deps
        for k in writes:
            self.last_write[k] = ins
            self.readers[k] = []
        for k in reads:
            if k not in writes:
                self.readers.setdefault(k, []).append(ins)
        for d in deps:
            if d.is_dma:
                ins.dma_waits[d.sem] = self.dma_cnt[d.sem[1]]
        if dma is not None:
            c = self.dma_cnt.get(dma, 0) + 16
            self.dma_cnt[dma] = c
            ins.sem = ("dma", dma)
            ins.val = c
        self.streams[eng].append(ins)
        return ins

    def emit(self, block_engines):
        nc = self.nc
        for e in ENGS:
            for ins in self.streams[e]:
                for d in ins.deps:
                    if d.eng == "pe" and ins.eng == "pe":
                        continue
                    d.signal = True
        sems = {}

        def get_sem(key):
            if key not in sems:
                sems[key] = nc.alloc_semaphore("s_%s_%s" % (key[0], key[1]))
            return sems[key]
        for e in ENGS:
            cnt = {}
            for ins in self.streams[e]:
                if ins.is_dma:
                    get_sem(ins.sem)
                    continue
                if ins.signal:
                    key = (e, ins.epoch)
                    cnt[key] = cnt.get(key, 0) + 1
                    ins.sem = key
                    ins.val = cnt[key]
                    get_sem(key)
        for e in ENGS:
            stream = self.streams[e]
            if not stream:
                continue

            def body(engine, stream=stream, e=e):
                waited = {}
                for ins in stream:
                    need = {}
                    for d in ins.deps:
                        if d.eng == "pe" and e == "pe":
                            continue
                        v = ins.dma_waits[d.sem] if d.is_dma else d.val
                        if v > need.get(d.sem, 0):
                            need[d.sem] = v
                    for key, v in need.items():
                        if waited.get(key, 0) >= v:
                            continue
                        waited[key] = v
                        engine.wait_ge(sems[key], v)
                    if ins.fn is None:
                        continue
                    bi = ins.fn(engine)
                    if ins.is_dma:
                        bi.then_inc(sems[ins.sem], 16)
                    elif ins.signal:
                        bi.then_inc(sems[ins.sem], 1)
            block_engines[e](body)


def unit_cols():
    o_val, o_gate, o_z, o_q, o_k, o_v, o_az, o_iq, o_ik, o_iw, o_ga, o_gb = (
        0, 512, 1024, 1536, 2048, 2112, 2176, 2688, 3200, 3264, 3272, 4296)
    units = []
    units.append(list(range(o_k, o_k + 64)) + list(range(o_ik, o_ik + 64)))
    for h in range(8):
        units.append(list(range(o_q + 64 * h, o_q + 64 * h + 64)) + list(range(o_iq + 64 * h, o_iq + 64 * h + 64)))
    for i in range(4):
        units.append(list(range(o_val + 128 * i, o_val + 128 * i + 128)))
        units.append(list(range(o_gate + 128 * i, o_gate + 128 * i + 128)))
    for i in range(4):
        units.append(list(range(o_z + 128 * i, o_z + 128 * i + 128)))
    for j in range(4):
        units.append(list(range(o_az + 128 * j, o_az + 128 * j + 128)))
    for e in range(8):
        units.append(list(range(o_ga + 128 * e, o_ga + 128 * e + 128)))
        units.append(list(range(o_gb + 128 * e, o_gb + 128 * e + 128)))
    assert len(units) == NUNIT
    vcols = list(range(o_v, o_v + 64)) + list(range(o_iw, o_iw + 8))
    return units, vcols


def build_nc(nchunk=NCHUNK, debug=None):
    nc = bass.Bass("TRN2", target_bir_lowering=False)
    dt_in = lambda name, shape: nc.dram_tensor(name, list(shape), F32, kind="ExternalInput").ap()
    x_d = dt_in("x", [TPC, D])
    p_d = dt_in("p", [TPC, 256])
    wu_d = dt_in("wu", [NUNIT, 128, 8, 128])
    wv_d = dt_in("wv", [128, 8, 72])
    wa_d = dt_in("wa", [128, 4, 1024])
    wb_d = dt_in("wb", [64, 8, 1024])
    wo_d = dt_in("wo", [128, 8, 1024])
    wg_d = dt_in("wg", [128, 8, 1024])
    wp_d = dt_in("wp", [128, 2, 1024])
    cvec_d = dt_in("cvec", [128, 4, 3])
    gvec_d = dt_in("gvec", [128, 3, 1024])
    ident_d = dt_in("ident", [128, 128])
    cb_d = dt_in("cb", [128, 128])
    augk_d = dt_in("augk", [4, L])
    augq_d = dt_in("augq", [4, 8, CH])
    diag_d = dt_in("diagw", [4, 128, 31, 128])
    ctab_d = dt_in("ctab", [128, NIT + 1])
    dscr = nc.dram_tensor("dscr", [4, 128, 31, 128], BF16).ap()
    y_d = nc.dram_tensor("y", [TPC, D], F32, kind="ExternalOutput").ap()
    dbg_out = {}
    if debug:
        for name, shape in debug.items():
            dbg_out[name] = nc.dram_tensor("dbg_" + name, list(shape), F32, kind="ExternalOutput").ap()

    S = Sched(nc)

    base = [(nc.sbuf_base + 63) // 64 * 64]
    top = nc.sbuf_top

    def alloc(name, shape, dtype, at=None):
        nbytes = int(np.prod(shape[1:])) * (2 if dtype == BF16 else 4)
        nbytes = (nbytes + 63) // 64 * 64
        if at is None:
            off = base[0]
            base[0] += nbytes
            assert base[0] <= top, (name, base[0], top)
        else:
            off = at
        return nc.alloc_sbuf_tensor_at(name, list(shape), dtype, offset=off), off

    WR, _ = alloc("WR", [128, RING, 8, 128], BF16)
    Wv, _ = alloc("Wv", [128, 8, 72], BF16)
    WA, _ = alloc("WA", [128, 4, 1024], BF16)
    WB, _ = alloc("WB", [128, 8, 1024], BF16)
    WO, _ = alloc("WO", [128, 8, 1024], BF16)
    WG, _ = alloc("WG", [128, 8, 1024], BF16)
    WP, _ = alloc("WP", [128, 2, 1024], BF16)
    identb, _ = alloc("identb", [128, 128], BF16)
    identf, _ = alloc("identf", [128, 128], F32)
    bigI, _ = alloc("bigI", [128, 128], BF16)
    CB, _ = alloc("CB", [128, 128], F32)
    onesf, _ = alloc("onesf", [128, 128], F32)
    onesrow, _ = alloc("onesrow", [128, 64], F32)
    Gb, _ = alloc("Gb", [128, 1024], F32)
    cvec, _ = alloc("cvec", [128, 4, 3], F32)
    xin, _ = alloc("xin", [128, 4, 1024], F32)
    hb, hb_off = alloc("hb", [128, 4, 1024], BF16)
    Sc2, _ = alloc("Sc2", [128, 2048], F32, at=hb_off)
    hT, _ = alloc("hT", [128, 8, 512], BF16)
    ubuf, _ = alloc("ubuf", [128, 4, 542], BF16)
    yaT, _ = alloc("yaT", [128, 4, 512], BF16)
    c1, _ = alloc("c1", [128, 4, 512], F32)
    scrt, scr_off = alloc("scrt", [128, 4096], F32)
    Sc, _ = alloc("Sc", [128, 2048], F32, at=scr_off)
    Mk, _ = alloc("Mk", [128, 2048], BF16, at=scr_off + 8192)
    Rl, _ = alloc("Rl", [128, 2, 512], F32, at=scr_off + 12288)
    Jk, _ = alloc("Jk", [128, 2048], BF16, at=scr_off + 12288)
    pin, _ = alloc("pin", [128, 4, 256], F32, at=scr_off)
    obuf, _ = alloc("obuf", [128, 4, 1024], F32, at=scr_off)
    pb, _ = alloc("pb", [128, 4, 256], BF16, at=scr_off + 4096)
    pT, _ = alloc("pT", [128, 2, 512], BF16, at=scr_off + 6144)
    QA, qa_off = alloc("QA", [128, 8, 512], BF16)
    QI, _ = alloc("QI", [128, 8, 512], BF16)
    mrg = QI
    KA, _ = alloc("KA", [128, L], BF16)
    KI, _ = alloc("KI", [128, L], BF16)
    VAf, _ = alloc("VAf", [128, 17 * 66], BF16)
    VA = VAf[:, 0:16 * 66].rearrange("p (k c) -> p k c", c=66)
    idxw, _ = alloc("idxw", [128, 4, 8], F32)
    maskT, mt_off = alloc("maskT", [128, 16, 512], BF16)
    diag = [alloc("diag0", [128, 31, 128], BF16, at=mt_off)[0],
            alloc("diag1", [128, 31, 128], BF16, at=mt_off + 8192)[0]]
    Pb, _ = alloc("Pb", [128, 4, 512], BF16)
    ybT, _ = alloc("ybT", [128, 8, 512], BF16)
    tmpf, _ = alloc("tmpf", [128, 3, 512], F32)
    st, _ = alloc("st", [128, 64], F32)
    bs, _ = alloc("bs", [128, 16], F32)
    ctab, _ = alloc("ctab", [128, NIT + 1], F32)
    wtab, _ = alloc("wtab", [128, NIT + 1], F32)
    wtab2, _ = alloc("wtab2", [128, NIT + 1], F32)
    nwtab2, _ = alloc("nwtab2", [128, NIT + 1], F32)
    bs2, _ = alloc("bs2", [128, 16], F32)

    pm = [nc.alloc_psum_tensor("pm0", [128, 512], F32), nc.alloc_psum_tensor("pm1", [128, 512], F32)]
    psc = [nc.alloc_psum_tensor("psc0", [128, 512], F32), nc.alloc_psum_tensor("psc1", [128, 512], F32)]
    pso = nc.alloc_psum_tensor("pso", [128, 512], F32)
    psx = nc.alloc_psum_tensor("psx", [128, 512], F32)
    psy = nc.alloc_psum_tensor("psy", [128, 512], F32)
    pst = nc.alloc_psum_tensor("pst", [128, 1024], BF16)

    def mm(out, lhsT, rhs, start, stop, reads, writes):
        S.op("pe", lambda e: e.matmul(out, lhsT=lhsT, rhs=rhs, start=start, stop=stop), reads, writes)

    def tr(out, in_, reads, writes):
        S.op("pe", lambda e: e.transpose(out=out, in_=in_, identity=identb[:, :]), list(reads) + ["identb"], writes)

    def act(out, in_, func, reads, writes, bias=None, scale=None, accum=None):
        kw = {}
        if bias is not None:
            kw["bias"] = bias
        if scale is not None:
            kw["scale"] = scale
        if accum is not None:
            kw["accum_out"] = accum
        S.op("act", lambda e: e.activation(out=out, in_=in_, func=func, **kw), reads, writes)

    def tt(eng, out, in0, in1, op, reads, writes):
        S.op(eng, lambda e: e.tensor_tensor(out=out, in0=in0, in1=in1, op=op), reads, writes)

    def ts(eng, out, in0, s1, s2, op0, op1, reads, writes, accum=None):
        if accum is not None:
            S.op(eng, lambda e: e.tensor_scalar(out=out, in0=in0, scalar1=s1, scalar2=s2, op0=op0, op1=op1, accum_out=accum), reads, writes)
        elif op1 is None:
            S.op(eng, lambda e: e.tensor_scalar(out=out, in0=in0, scalar1=s1, scalar2=None, op0=op0), reads, writes)
        else:
            S.op(eng, lambda e: e.tensor_scalar(out=out, in0=in0, scalar1=s1, scalar2=s2, op0=op0, op1=op1), reads, writes)

    def stt(out, in0, scalar, in1, op0, op1, reads, writes):
        S.op("dve", lambda e: e.scalar_tensor_tensor(out=out, in0=in0, scalar=scalar, in1=in1, op0=op0, op1=op1), reads, writes)

    def cp(eng, out, in_, reads, writes):
        if eng == "act":
            S.op("act", lambda e: e.copy(out=out, in_=in_), reads, writes)
        else:
            S.op(eng, lambda e: e.tensor_copy(out=out, in_=in_), reads, writes)

    def memset(eng, ap, val, writes):
        S.op(eng, lambda e: e.memset(ap, val), (), writes)

    def dma(eng, out, in_, reads, writes, sem, **kw):
        return S.op(eng, lambda e: e.dma_start(out=out, in_=in_, **kw), reads, writes, dma=sem)

    out_dmas = []

    def dbg(name, src_ap, reads):
        if name in dbg_out:
            out_dmas.append(dma("pool", dbg_out[name], src_ap, reads, [], "out", max_dma_last_dim=2048))

    CAST = dict(max_dma_last_dim=4096)
    unit_state = {"next": 0}
    total_units = nchunk * NUNIT

    def issue_unit():
        g = unit_state["next"]
        if g >= total_units:
            return
        unit_state["next"] = g + 1
        slot = g % RING
        u = g % NUNIT
        dma("pool", WR[:, slot, :, :], wu_d[u, :, :, :], [], ["WR%d" % slot], "wr%d" % slot, **CAST)

    issue_unit()
    dma("pool", Wv[:, :, :], wv_d[:, :, :], [], ["Wv"], "w_tail", **CAST)
    for _ in range(RING - 1):
        issue_unit()
    cons = {"g": 0}

    def take_unit():
        g = cons["g"]
        cons["g"] = g + 1
        return g % RING

    dma("sp", identf[:, :], ident_d[:, :], [], ["identf"], "consts")
    dma("sp", cvec[:, :, :], cvec_d[:, :, :], [], ["cvec"], "consts")
    dma("sp", CB[:, :], cb_d[:, :], [], ["CB"], "consts")
    dma("sp", ctab[:, :], ctab_d[:, :], [], ["ctab"], "consts")
    cp("dve", identb[:, :], identf[:, :], ["identf"], ["identb"])
    ts("dve", bigI[:, :], identf[:, :], 29952.0, None, ALU.mult, None, ["identf"], ["bigI"])
    memset("dve", onesf[:, :], 1.0 / 512.0, ["onesf"])
    memset("dve", onesrow[:, :], 1.0, ["onesrow"])
    memset("dve", VAf[:, :], 0.0, ["VA"])
    memset("dve", VA[:, :, 64:66], 1.0, ["VA"])
    memset("dve", KA[64:128, :], 0.0, ["KA"])
    memset("dve", QA[64:128, :, :], 0.0, ["QA"])
    memset("dve", KI[0:64, :], 0.0, ["KI"])
    for i4 in range(4):
        dma("pool", dscr[i4], diag_d[i4], [], ["dscr%d" % i4], "consts2", **CAST)
    dma("pool", KA[64:68, :], augk_d[:, :], [], ["KA"], "consts2", **CAST)
    dma("pool", QA[64:68, :, :], augq_d[:, :, :], [], ["QA"], "consts2", **CAST)

    def load_tail_ab():
        dma("pool", WA[:, :, :], wa_d[:, :, :], [], ["WA"], "w_tail", **CAST)
        dma("pool", WB[0:64, :, :], wb_d[:, :, :], [], ["WB"], "w_tail", **CAST)

    def load_tail_rest():
        dma("pool", WO[:, :, :], wo_d[:, :, :], [], ["WO"], "w_tail", **CAST)
        dma("pool", WG[:, :, :], wg_d[:, :, :], [], ["WG"], "w_tail", **CAST)
        dma("pool", WP[:, :, :], wp_d[:, :, :], [], ["WP"], "w_tail", **CAST)

    SQK = ["scrM", "Rl0", "Rl1"]
    pmi = [0]

    def next_pm():
        pmi[0] ^= 1
        return pmi[0]

    def proj_unit(out_rows=128, lhs_cols=(0, 128), release=True, slot=None, bank=None):
        if slot is None:
            slot = take_unit()
        c0, c1_ = lhs_cols
        if bank is None:
            i = next_pm()
            bt, bk = pm[i], "pm%d" % i
        else:
            i = None
            bt, bk = bank
        for k in range(8):
            mm(bt[0:out_rows, :], WR[:, slot, k, c0:c1_], hT[:, k, :], k == 0, k == 7,
               ["WR%d" % slot, "hT0", "hT1", "hT2", "hT3"], [bk])
        if release:
            issue_unit()
        return i, slot

    def rstd4():
        act(st[:, 4:8], st[:, 0:4], AF.Sqrt, ["st"] + STK, ["st"], bias=EPS, scale=1.0 / D)
        S.op("dve", lambda e: e.reciprocal(out=st[:, 8:12], in_=st[:, 4:8]), ["st"], ["st"])

    def load_gain(g):
        dma("sp", Gb[:, :], gvec_d[:, g, :], [], ["G"], "gld")

    def sumsq(j, junk, jkeys):
        if j % 2 == 0:
            act(junk, xin[:, j, :], AF.Square, ["xin"], jkeys + ["st%d" % j], accum=st[:, j:j + 1])
        else:
            S.op("dve", lambda e: e.scalar_tensor_tensor(out=junk, in0=xin[:, j, :], scalar=1.0, in1=xin[:, j, :],
                                                         op0=ALU.mult, op1=ALU.mult, accum_out=st[:, j:j + 1]),
                 ["xin"], jkeys + ["st%d" % j])

    STK = ["st0", "st1", "st2", "st3"]

    def rmsnorm_to_hT(gidx, tag, after=None):
        for j in range(4):
            sumsq(j, hb[:, j, :], ["hb%d" % j])
        rstd4()
        for j in range(4):
            stt(hb[:, j, :], xin[:, j, :], st[:, 8 + j:9 + j], Gb[:, :], ALU.mult, ALU.mult,
                ["xin", "st", "G", "hb", "hb%d" % j], ["hb%d" % j])
        load_gain(gidx + 1)
        for j in range(4):
            for k in range(8):
                tr(pst[:, k * 128:(k + 1) * 128], hb[:, j, k * 128:(k + 1) * 128], ["hb", "hb%d" % j], ["pst"])
            pv8 = pst[:, 0:1024].rearrange("p (k c) -> p k c", k=8)
            cp("act", hT[:, :, j * 128:(j + 1) * 128], pv8, ["pst"], ["hT%d" % j])
            if after is not None and j >= 1:
                after(j - 1)
        if after is not None:
            after(3)

    for c in range(nchunk):
        S.next_epoch()
        b = c // NCL
        cl = c % NCL
        tok0 = c * CH
        t0b = cl * CH
        first = (c == 0)

        load_gain(0)
        dma("sp", xin[:, :, :], x_d[tok0:tok0 + CH, :].rearrange("(j p) d -> p j d", p=128), [], ["xin"], "xin")
        rmsnorm_to_hT(0, "n1")
        if first:
            dbg("hT", hT[:, :, :], ["hT0", "hT1", "hT2", "hT3"])

        i, _ = proj_unit()
        cp("act", KA[0:64, t0b:t0b + CH], pm[i][0:64, :], ["pm%d" % i], ["KA"])
        cp("dve", KI[64:128, t0b:t0b + CH], pm[i][64:128, :], ["pm%d" % i], ["KI"])
        for j in range(4):
            for k in range(8):
                mm(psx[:, j * 72:(j + 1) * 72], hT[:, k, j * 128:(j + 1) * 128], Wv[:, k, :], k == 0, k == 7,
                   ["hT%d" % j, "Wv"], ["psx"])
        pv = psx[:, 0:288].rearrange("p (j c) -> p j c", c=72)
        cp("dve", VA[:, cl * 4:cl * 4 + 4, 0:64], pv[:, :, 0:64], ["psx"], ["VA"])
        cp("dve", idxw[:, :, :], pv[:, :, 64:72], ["psx"], ["idxw"])

        memset("dve", QI[0:64, :, :], 0.0, ["QI"])
        for h in range(8):
            i, _ = proj_unit()
            cp("act", QA[0:64, h, :], pm[i][0:64, :], ["pm%d" % i], ["QA"])
            cp("dve", QI[64:128, h, :], pm[i][64:128, :], ["pm%d" % i], ["QI"])
        if first:
            dbg("KA", KA[0:68, 0:512], ["KA"])
            dbg("QA", QA[0:68, :, :], ["QA"])
            dbg("QI", QI[64:128, :, :], ["QI"])
            dbg("VA", VA[:, 0:4, :], ["VA"])
            dbg("idxw", idxw[:, :, :], ["idxw"])

        def load_diag(i4):
            dma("sp", diag[i4 % 2][:, :, :], dscr[i4], ["dscr%d" % i4], ["mT%d" % (i4 % 2)], "dg%d" % (i4 % 2))
        vg_banks = [((pm[0], "pm0"), (pm[1], "pm1")), ((psc[0], "psc0"), (psc[1], "psc1"))]

        def proj_vg(i4):
            bv, bg = vg_banks[i4 % 2]
            proj_unit(bank=bv)
            proj_unit(bank=bg)

        def s4_gen():
            if cl == 0:
                memset("dve", ubuf[:, :, 0:30], 0.0, ["ubuf"])
            else:
                cp("dve", ubuf[:, :, 0:30], ubuf[:, :, 512:542], ["ubuf"], ["ubuf"])
            load_diag(0)
            load_diag(1)
            proj_vg(0)
            yield
            for i4 in range(4):
                dkey = "mT%d" % (i4 % 2)
                dg = diag[i4 % 2]
                (vt, vk), (gt, gk) = vg_banks[i4 % 2]
                if i4 + 1 < 4:
                    proj_vg(i4 + 1)
                    yield
                tk = "tmpf%d" % (i4 % 2)
                act(tmpf[:, i4 % 2, :], gt[:, :], AF.Sigmoid, [gk], [tk])
                tt("dve", ubuf[:, i4, 30:542], vt[:, :], tmpf[:, i4 % 2, :], ALU.mult, [vk, tk], ["ubuf"])
                yield
                for j in range(31):
                    mm(pso[:, :], dg[:, j, :], ubuf[:, i4, j:j + 512], j == 0, j == 30, [dkey, "ubuf"], ["pso"])
                if i4 + 2 < 4:
                    load_diag(i4 + 2)
                yield
                act(c1[:, i4, :], pso[:, :], AF.Identity, ["pso", "cvec"], ["c1"], bias=cvec[:, i4, 0:1])
                act(tmpf[:, 2, :], pso[:, :], AF.Square, ["pso", "cvec"], ["tmpf2"], bias=cvec[:, i4, 0:1])
                mm(psx[:, :], onesf[:, :], c1[:, i4, :], i4 == 0, i4 == 3, ["onesf", "c1"], ["psx"])
                mm(psy[:, :], onesf[:, :], tmpf[:, 2, :], i4 == 0, i4 == 3, ["onesf", "tmpf2"], ["psy"])
                yield
            act(tmpf[:, 1, :], psx[:, :], AF.Square, ["psx"], ["tmpf1"])
            tt("dve", tmpf[:, 1, :], psy[:, :], tmpf[:, 1, :], ALU.subtract, ["psy", "tmpf1"], ["tmpf1"])
            act(tmpf[:, 1, :], tmpf[:, 1, :], AF.Sqrt, ["tmpf1"], ["tmpf1"], bias=EPS, scale=1.0)
            S.op("dve", lambda e: e.reciprocal(out=tmpf[:, 1, :], in_=tmpf[:, 1, :]), ["tmpf1"], ["tmpf1"])
            yield
            for i4 in range(4):
                iz, _ = proj_unit()
                tt("dve", c1[:, i4, :], c1[:, i4, :], psx[:, :], ALU.subtract, ["c1", "psx"], ["c1"])
                tt("dve", c1[:, i4, :], c1[:, i4, :], tmpf[:, 1, :], ALU.mult, ["c1", "tmpf1"], ["c1"])
                yield
                act(tmpf[:, 0, :], c1[:, i4, :], AF.Silu, ["c1", "cvec"], ["tmpf0"],
                    bias=cvec[:, i4, 2:3], scale=cvec[:, i4, 1:2])
                act(tmpf[:, 2, :], pm[iz][:, :], AF.Silu, ["pm%d" % iz], ["tmpf2"])
                tt("dve", yaT[:, i4, :], tmpf[:, 0, :], tmpf[:, 2, :], ALU.mult, ["tmpf0", "tmpf2"], ["yaT"])
                yield
            for jz in range(4):
                slot = take_unit()
                for hh in range(2):
                    h = 2 * jz + hh
                    i, _ = proj_unit(out_rows=64, lhs_cols=(hh * 64, hh * 64 + 64), release=(hh == 1), slot=slot)
                    act(ybT[0:64, h, :], pm[i][0:64, :], AF.Silu, ["pm%d" % i], ["ybT"])
                    yield

        s4 = s4_gen()

        def s4_step(n):
            for _ in range(n):
                try:
                    next(s4)
                except StopIteration:
                    return

        if first:
            load_tail_ab()
        ri = [0]

        def indexer(j, Sb, skey):
            qt = cl * 4 + j
            nk = (qt + 1) * 128
            nseg = (nk + 511) // 512
            for h in range(8):
                for sg in range(nseg):
                    w = min(512, nk - sg * 512)
                    pi = (h * nseg + sg) % 2
                    mm(psc[pi][:, 0:w], QI[:, h, j * 128:(j + 1) * 128], KI[:, sg * 512:sg * 512 + w],
                       True, True, ["QI", "KI"], ["psc%d" % pi])
                    ri[0] ^= 1
                    r = ri[0]
                    act(Rl[:, r, 0:w], psc[pi][:, 0:w], AF.Relu, ["psc%d" % pi], ["Rl%d" % r])
                    dst = Sb[:, sg * 512:sg * 512 + w]
                    if h == 0:
                        ts("dve", dst, Rl[:, r, 0:w], idxw[:, j, 0:1], None, ALU.mult, None,
                           ["Rl%d" % r, "idxw"], [skey])
                    else:
                        stt(dst, Rl[:, r, 0:w], idxw[:, j, h:h + 1], dst, ALU.mult, ALU.add,
                            ["Rl%d" % r, "idxw", skey], [skey])

        def bracket(Sb, skey, nk, b_, bkey):
            S.op("dve", lambda e: e.tensor_reduce(out=b_[:, 0:1], in_=Sb[:, 0:nk], axis=AX.X, op=ALU.max,
                                                  apply_absolute_value=True), [skey], [bkey])
            ts("dve", b_[:, 2:3], b_[:, 0:1], 2.002, 2e-3, ALU.mult, ALU.add, [bkey], [bkey])
            memset("dve", b_[:, 3:4], 0.0, [bkey])

        def mask_T(j, qt):
            for g0 in range(0, qt + 1, 8):
                g1 = min(qt + 1, g0 + 8)
                for kb in range(g0, g1):
                    tr(pst[:, (kb - g0) * 128:(kb - g0 + 1) * 128], Mk[:, kb * 128:(kb + 1) * 128], ["scrM"], ["pst"])
                mkey = "mT0" if g0 == 0 else "mT1"
                cp("act", maskT[:, g0:g1, j * 128:(j + 1) * 128],
                   pst[:, 0:(g1 - g0) * 128].rearrange("p (k c) -> p k c", c=128), ["pst"], [mkey])

        for pair_i, (jA, jB) in enumerate(((0, 1), (2, 3))):
            qtA, qtB = cl * 4 + jA, cl * 4 + jB
            nkA, nkB = (qtA + 1) * 128, (qtB + 1) * 128
            bis = qtA >= 2
            indexer(jA, Sc, "scrA")
            indexer(jB, Sc2, "hb")
            if bis:
                bracket(Sc, "scrA", nkA, bs, "bs")
                ts("dve", wtab[:, :], ctab[:, :], bs[:, 2:3], None, ALU.mult, None, ["bs", "ctab"], ["wtab"])
                bracket(Sc2, "hb", nkB, bs2, "bs2")
                ts("dve", wtab2[:, :], ctab[:, :], bs2[:, 2:3], None, ALU.mult, None, ["bs2", "ctab"], ["wtab2"])
                ts("dve", nwtab2[:, :], ctab[:, :], bs2[:, 2:3], -1.0, ALU.mult, ALU.mult, ["bs2", "ctab"], ["nwtab2"])
            tt("dve", Sc[:, qtA * 128:nkA], Sc[:, qtA * 128:nkA], CB[:, :], ALU.add, ["scrA", "CB"], ["scrA"])
            tt("dve", Sc2[:, qtB * 128:nkB], Sc2[:, qtB * 128:nkB], CB[:, :], ALU.add, ["hb", "CB"], ["hb"])
            if first and jA == 2:
                dbg("Sc", Sc[:, 0:384], ["scrA"])
            if bis:
                jk = ["Rl0", "Rl1"]
                sbias = float(nkB) - 510.5
                if pair_i == 0 and not INTERLEAVE:
                    s4_step(1000)
                for it in range(NIT):
                    ts("dve", Mk[:, 0:nkA], Sc[:, 0:nkA], bs[:, 3:4], 0.0, ALU.is_ge, ALU.add,
                       ["scrA", "bs", "scrM"], ["scrM", "bs"], accum=bs[:, 4:5])
                    ts("dve", bs[:, 5:6], bs[:, 4:5], 255.5, wtab[:, it:it + 1], ALU.is_ge, ALU.mult, ["bs", "wtab"], ["bs"])
                    nxt = it + 1 if it + 1 < NIT else it
                    stt(bs[:, 3:4], bs[:, 5:6], wtab[:, nxt:nxt + 1], bs[:, 3:4], ALU.subtract, ALU.add, ["bs", "wtab"], ["bs"])
                    act(Jk[:, 0:nkB], Sc2[:, 0:nkB], AF.Sign, ["hb", "bs2"], jk + ["bs2"], bias=bs2[:, 3:4], scale=1.0,
                        accum=bs2[:, 4:5])
                    act(bs2[:, 5:6], bs2[:, 4:5], AF.Sign, ["bs2"], ["bs2"], bias=sbias, scale=1.0)
                    act(bs2[:, 3:4], bs2[:, 5:6], AF.Identity, ["bs2", "nwtab2"], ["bs2"], bias=bs2[:, 3:4],
                        scale=nwtab2[:, it + 1:it + 2])
                    if pair_i == 0 and INTERLEAVE:
                        s4_step(2)
                if pair_i == 0:
                    s4_step(1000)
                act(bs2[:, 6:7], bs2[:, 3:4], AF.Identity, ["bs2", "nwtab2"], ["bs2"], bias=nwtab2[:, NIT:NIT + 1], scale=-1.0)
                ts("dve", Mk[:, 0:nkA], Sc[:, 0:nkA], bs[:, 3:4], 1.0, ALU.is_ge, ALU.subtract, ["scrA", "bs", "scrM"], ["scrM"])
                if first and jA == 2:
                    dbg("thr", bs[:, 0:16], ["bs"])
            else:
                if pair_i == 0:
                    s4_step(1000)
                ts("dve", Mk[:, 0:nkA], Sc[:, 0:nkA], -1e29, 1.0, ALU.is_ge, ALU.subtract, ["scrA", "scrM"], ["scrM"])
            mask_T(jA, qtA)
            if bis:
                ts("dve", Mk[:, 0:nkB], Sc2[:, 0:nkB], bs2[:, 6:7], 1.0, ALU.is_ge, ALU.subtract, ["hb", "bs2", "scrM"], ["scrM"])
            else:
                ts("dve", Mk[:, 0:nkB], Sc2[:, 0:nkB], -1e29, 1.0, ALU.is_ge, ALU.subtract, ["hb", "scrM"], ["scrM"])
            mask_T(jB, qtB)

        if first:
            load_tail_rest()
        nkb = (cl + 1) * 4
        items = [(h, kb) for h in range(8) for kb in range(nkb)]
        nit = len(items)
        accs = [(pso, "pso"), (psy, "psy")]
        bcs = [(psx, "psx"), (psx, "psx")]
        lbk = [(psc[0], "psc0"), (psc[1], "psc1"), (pm[0], "pm0"), (pm[1], "pm1")]
        LOOK = 4
        rsl = [0, 2]

        def q0_of(kb):
            return max(0, kb - cl * 4) * 128

        def emit_L(idx):
            h, kb = items[idx]
            q0 = q0_of(kb)
            lt, lk = lbk[idx % LOOK]
            mm(lt[:, q0:CH], KA[:, kb * 128:(kb + 1) * 128], QA[:, h, q0:CH], True, False,
               ["KA", "QA"], [lk])
            mm(lt[:, q0:CH], bigI[:, :], maskT[:, kb, q0:CH], False, True,
               ["bigI", "mT0" if kb < 8 else "mT1"], [lk])

        def emit_norm(h):
            acc, akey = accs[h % 2]
            bc, bkey = bcs[h % 2]
            r = rsl[h % 2]
            mm(bc[0:64, :], onesrow[64:65, 0:64], tmpf[64:65, r, :], True, True, ["onesrow", "tmpf%d" % r], [bkey])
            tt("dve", tmpf[0:64, 1, :], acc[0:64, :], ybT[0:64, h, :], ALU.mult, [akey, "ybT"], ["tmpf1"])
            tt("dve", ybT[0:64, h, :], tmpf[0:64, 1, :], bc[0:64, :], ALU.mult, ["tmpf1", bkey], ["ybT"])

        deferred = []
        for i_ in range(min(LOOK, nit)):
            emit_L(i_)
        for idx in range(nit):
            h, kb = items[idx]
            q0 = q0_of(kb)
            lt, lk = lbk[idx % LOOK]
            pr = idx % 4
            slope = 2.0 ** (-(h + 1))
            ebias = -slope * CH * cl
            mkey = "mT0" if kb < 8 else "mT1"
            acc, akey = accs[h % 2]
            act(Pb[:, pr, q0:CH], lt[:, q0:CH], AF.Exp, [lk], ["Pb%d" % pr], bias=ebias, scale=0.125)
            if idx + LOOK < nit:
                emit_L(idx + LOOK)
            mm(acc[:, q0:CH], VAf[:, kb * 66:kb * 66 + 128], Pb[:, pr, q0:CH], kb == 0, kb == nkb - 1, ["VA", "Pb%d" % pr], [akey])
            while deferred and deferred[0][0] <= idx:
                emit_norm(deferred.pop(0)[1])
            if kb == nkb - 1:
                r = rsl[h % 2]
                S.op("dve", lambda e, r=r, acc=acc: e.reciprocal(out=tmpf[64:65, r, :], in_=acc[64:65, :]), [akey], ["tmpf%d" % r])
                deferred.append((idx + 3, h))
        while deferred:
            emit_norm(deferred.pop(0)[1])
        if first:
            dbg("yaT", yaT[:, :, :], ["yaT"])
            dbg("ybT", ybT[0:64, :, :], ["ybT"])
            dbg("maskT", maskT[:, 0:4, :], ["mT0"])

        dma("sp", pin[:, :, :], p_d[tok0:tok0 + CH, :].rearrange("(j p) d -> p j d", p=128), [], ["scrA"], "pin")
        cp("dve", pb[:, :, :], pin[:, :, :], ["scrA"], ["scrA"])
        for j in range(4):
            for kk in range(2):
                tr(pst[:, (j * 2 + kk) * 128:(j * 2 + kk + 1) * 128], pb[:, j, kk * 128:(kk + 1) * 128], ["scrA"], ["pst"])
        for kk in range(2):
            cp("act", pT[:, kk, :].rearrange("p (j c) -> p j c", j=4),
               pst[:, 0:1024].rearrange("p (j k c) -> p j k c", j=4, k=2)[:, :, kk, :], ["pst"], ["scrA"])
        for e8 in range(8):
            iga, _ = proj_unit()
            igb, _ = proj_unit()
            for kc in range(4):
                mm(psx[:, :], WA[:, kc, e8 * 128:(e8 + 1) * 128], yaT[:, kc, :], kc == 0, kc == 3, ["WA", "yaT"], ["psx"])
            for h in range(8):
                mm(psy[:, :], WB[0:64, h, e8 * 128:(e8 + 1) * 128], ybT[0:64, h, :], h == 0, h == 7, ["WB", "ybT"], ["psy"])
            act(tmpf[:, 0, :], pm[iga][:, :], AF.Sigmoid, ["pm%d" % iga], ["tmpf0"])
            act(tmpf[:, 1, :], pm[igb][:, :], AF.Sigmoid, ["pm%d" % igb], ["tmpf1"])
            tt("dve", tmpf[:, 0, :], tmpf[:, 0, :], psx[:, :], ALU.mult, ["tmpf0", "psx"], ["tmpf0"])
            tt("dve", tmpf[:, 1, :], tmpf[:, 1, :], psy[:, :], ALU.mult, ["tmpf1", "psy"], ["tmpf1"])
            tt("dve", mrg[:, e8, :], tmpf[:, 0, :], tmpf[:, 1, :], ALU.add, ["tmpf0", "tmpf1"], ["QI"])
        if first:
            dbg("mrg", mrg[:, :, :], ["QI"])

        for j in range(4):
            for hf in range(2):
                i = next_pm()
                for k in range(8):
                    mm(pm[i][:, :], mrg[:, k, j * 128:(j + 1) * 128], WO[:, k, hf * 512:(hf + 1) * 512], k == 0, k == 7,
                       ["QI", "WO"], ["pm%d" % i])
                tt("dve", xin[:, j, hf * 512:(hf + 1) * 512], xin[:, j, hf * 512:(hf + 1) * 512], pm[i][:, :], ALU.add,
                   ["xin", "pm%d" % i], ["xin"])
        def ple_tile(j):
            for hf in range(2):
                i = next_pm()
                for k in range(8):
                    mm(pm[i][:, :], hT[:, k, j * 128:(j + 1) * 128], WG[:, k, hf * 512:(hf + 1) * 512], k == 0, k == 7,
                       ["hT%d" % j, "WG"], ["pm%d" % i])
                for kk in range(2):
                    mm(psx[:, :], pT[:, kk, j * 128:(j + 1) * 128], WP[:, kk, hf * 512:(hf + 1) * 512], kk == 0, kk == 1,
                       ["scrA", "WP"], ["psx"])
                act(tmpf[:, 0, :], pm[i][:, :], AF.Sigmoid, ["pm%d" % i], ["tmpf0"])
                tt("dve", tmpf[:, 0, :], tmpf[:, 0, :], psx[:, :], ALU.mult, ["tmpf0", "psx"], ["tmpf0"])
                tt("dve", xin[:, j, hf * 512:(hf + 1) * 512], xin[:, j, hf * 512:(hf + 1) * 512], tmpf[:, 0, :], ALU.add,
                   ["xin", "tmpf0"], ["xin"])
        rmsnorm_to_hT(1, "n2", after=ple_tile)
        for j in range(4):
            sumsq(j, hb[:, j, :], ["hb%d" % j])
        rstd4()
        for j in range(4):
            stt(obuf[:, j, :], xin[:, j, :], st[:, 8 + j:9 + j], Gb[:, :], ALU.mult, ALU.mult, ["xin", "st", "G"],
                ["scrA"] + SQK)
        out_dmas.append(dma("sp", y_d[tok0:tok0 + CH, :].rearrange("(j p) d -> p j d", p=128), obuf[:, :, :],
                            ["scrA"] + SQK, [], "out"))

    fin = S.op("sp", None)
    fin.deps = list(out_dmas)
    for d in fin.deps:
        fin.dma_waits[d.sem] = S.dma_cnt["out"]

    with nc.Block() as block:
        S.emit({"pe": block.tensor, "act": block.scalar, "dve": block.vector, "pool": block.gpsimd, "sp": block.sync})
    return nc


def _host_consts():
    ident = np.eye(128, dtype=np.float32)
    tt_, ss_ = np.meshgrid(np.arange(128), np.arange(128), indexing="ij")
    cb = np.where(ss_ <= tt_, 0.0, -1e30).astype(np.float32)
    s = np.arange(L)
    augk = np.stack([s % 128, 128 * (s // 128), np.ones(L), np.ones(L)]).astype(np.float32)
    t = np.arange(CH)
    augq = np.zeros((4, 8, CH), np.float32)
    for h in range(8):
        sl = 2.0 ** (-(h + 1))
        augq[0, h] = 8 * sl
        augq[1, h] = 8 * sl
        augq[2, h] = -8 * sl * (t % 128)
        augq[3, h] = -8 * sl * 128 * (t // 128)
    return ident, cb, augk, augq


def _prep_inputs(x, p, norm_g, w_in, conv_w, conv_b, conv_ln_g, conv_ln_b, w_a_out, w_b_out,
                 w_o, ple_norm_g, w_ple_gate, w_ple_proj, final_g):
    f = lambda a: np.ascontiguousarray(np.asarray(a, dtype=np.float32))
    W = f(w_in)[0]
    units, vcols = unit_cols()
    wu = np.stack([W[:, cols].reshape(8, 128, 128).transpose(1, 0, 2) for cols in units])
    wv = W[:, vcols].reshape(8, 128, 72).transpose(1, 0, 2)
    wa = f(w_a_out)[0].reshape(4, 128, 1024).transpose(1, 0, 2)
    wb = f(w_b_out)[0].reshape(8, 64, 1024).transpose(1, 0, 2)
    wo = f(w_o)[0].reshape(8, 128, 1024).transpose(1, 0, 2)
    wg = f(w_ple_gate)[0].reshape(8, 128, 1024).transpose(1, 0, 2)
    wp = f(w_ple_proj)[0].reshape(2, 128, 1024).transpose(1, 0, 2)
    cw = f(conv_w)[0].reshape(31, 4, 128).transpose(2, 1, 0)
    cvec = np.stack([f(conv_b)[0].reshape(4, 128), f(conv_ln_g)[0].reshape(4, 128),
                     f(conv_ln_b)[0].reshape(4, 128)], axis=-1).transpose(1, 0, 2)
    gvec = np.broadcast_to(np.stack([f(norm_g)[0], f(ple_norm_g)[0], f(final_g)])[None], (128, 3, 1024))
    ident, cb, augk, augq = _host_consts()
    diagw = np.zeros((4, 128, 31, 128), np.float32)
    ar = np.arange(128)
    diagw[:, ar, :, ar] = cw.transpose(0, 1, 2)[ar][:, :, :].transpose(0, 1, 2)
    ctab = np.broadcast_to((2.0 ** -(np.arange(NIT + 1) + 1.0))[None, :], (128, NIT + 1))
    shared = dict(diagw=diagw, ctab=ctab, wu=wu, wv=wv, wa=wa, wb=wb, wo=wo, wg=wg, wp=wp, cvec=cvec, gvec=gvec,
                  ident=ident, cb=cb, augk=augk, augq=augq)
    shared = {k: np.ascontiguousarray(v, dtype=np.float32) for k, v in shared.items()}
    xs = f(x).reshape(NCORES, TPC, D)
    ps = f(p)[0].reshape(NCORES, TPC, 256)
    return [dict(shared, x=xs[i], p=ps[i]) for i in range(NCORES)]


def kernel(**inputs):
    in_maps = _prep_inputs(**inputs)
    nc = build_nc()
    res = run_bass_kernel_spmd(nc, in_maps, core_ids=list(range(NCORES)))
    out = np.stack([np.asarray(r["y"], dtype=np.float32) for r in res.results])
    return out.reshape(16, L, D)
```
